# Optimizing a Trainium2 kernel written in Bass

```python
import math
import jax
import jax.numpy as jnp
from jax import lax
import numpy as np

D_MODEL = 1024
BATCH = 16
SEQ = 2048
DEPTH = 4

GRID_W = 64
CTX_LEN = 256
N_MIXERS = 2
N_ATTN_LAYERS = (DEPTH + 1) // 2
N_MLSTM_LAYERS = DEPTH // 2
N_MOD = 9
D_FF = 2816
RMS_EPS = 1e-6
ATTN_HEADS = 8
ATTN_HEAD_DIM = D_MODEL // (2 * ATTN_HEADS)
ATTN_V_DIM = 2 * ATTN_HEAD_DIM
ROPE_THETA = 10000.0
Q_BLOCK = 128
SUBLN_EPS = 1e-5
MLSTM_INNER = 2 * D_MODEL
MLSTM_HEADS = 4
MLSTM_HEAD_DIM = MLSTM_INNER // MLSTM_HEADS
QKV_BLOCK = 4
CONV_W = 5
CHUNK = 128
HEAD_LN_EPS = 1e-5

kernel_name = 'hybrid_diffattn_mlstm_macaron_dit'


def rmsnorm(x, w, eps=RMS_EPS):
    xf = x.astype(jnp.float32)
    y = xf * lax.rsqrt(jnp.mean(xf * xf, axis=-1, keepdims=True) + eps)
    return (y * w.astype(jnp.float32)).astype(x.dtype)


def adaln_input(h, norm_w, mod, j):
    return rmsnorm(h, norm_w) * (1 + mod[:, :, 3 * j + 1]) + mod[:, :, 3 * j]


def swiglu(u, w_in, w_out):
    g, up = jnp.split(u @ w_in, 2, axis=-1)
    return (jax.nn.silu(g) * up) @ w_out


def half_ffn(h, norm_w, mod, j, w_in, w_out):
    return h + 0.5 * mod[:, :, 3 * j + 2] * swiglu(adaln_input(h, norm_w, mod, j), w_in, w_out)


def axial_rope(x):
    b_, n_tok, nh, two, dh = x.shape
    rows = n_tok // GRID_W
    row_pos = jnp.broadcast_to(jnp.arange(rows, dtype=jnp.float32)[:, None], (rows, GRID_W)).reshape(-1)
    col_pos = jnp.broadcast_to(jnp.arange(GRID_W, dtype=jnp.float32)[None, :], (rows, GRID_W)).reshape(-1)
    half = dh // 2
    inv_freq = 1.0 / (ROPE_THETA ** (jnp.arange(0, half, 2, dtype=jnp.float32) / half))
    ang = jnp.stack([row_pos[:, None] * inv_freq, col_pos[:, None] * inv_freq], axis=1)
    cos = jnp.cos(ang)[None, :, None, None].astype(x.dtype)
    sin = jnp.sin(ang)[None, :, None, None].astype(x.dtype)
    xr = x.reshape(b_, n_tok, nh, two, 2, 2, dh // 4)
    x1, x2 = xr[..., 0, :], xr[..., 1, :]
    out = jnp.stack([x1 * cos - x2 * sin, x2 * cos + x1 * sin], axis=-2)
    return out.reshape(x.shape)


def diff_attend(q, k, v, lam):
    s = jnp.einsum('bhcqd,bhckd->bhcqk', q, k).astype(jnp.float32) * (ATTN_HEAD_DIM ** -0.5)
    p = jax.nn.softmax(s, axis=-1)
    a = p[:, :, 0] - lam * p[:, :, 1]
    return jnp.einsum('bhqk,bhkv->bhqv', a.astype(v.dtype), v)


def diff_attention_mixer(u_lat, u_ctx, w_qkv, w_o, lam_vecs, subln_w, layer_idx, need_ctx):
    b_, n_tok, d = u_lat.shape
    lambda_init = 0.8 - 0.6 * math.exp(-0.3 * layer_idx)
    lv = lam_vecs.astype(jnp.float32)
    lam = jnp.exp(jnp.sum(lv[0] * lv[1])) - jnp.exp(jnp.sum(lv[2] * lv[3])) + lambda_init

    def project(u):
        n = u.shape[1]
        q, k, v = jnp.split(u @ w_qkv, 3, axis=-1)
        return (q.reshape(b_, n, ATTN_HEADS, 2, ATTN_HEAD_DIM),
                k.reshape(b_, n, ATTN_HEADS, 2, ATTN_HEAD_DIM),
                v.reshape(b_, n, ATTN_HEADS, ATTN_V_DIM).transpose(0, 2, 1, 3))

    def to_heads(t):
        return t.transpose(0, 2, 3, 1, 4)

    q_lat, k_lat, v_lat = project(u_lat)
    q_lat, k_lat = axial_rope(q_lat), axial_rope(k_lat)
    q_ctx, k_ctx, v_ctx = project(u_ctx)
    k_ctx, q_ctx = to_heads(k_ctx), to_heads(q_ctx)
    k_all = jnp.concatenate([k_ctx, to_heads(k_lat)], axis=3)
    v_all = jnp.concatenate([v_ctx, v_lat], axis=2)

    n_blk = n_tok // Q_BLOCK
    q_blocks = jnp.moveaxis(to_heads(q_lat).reshape(b_, ATTN_HEADS, 2, n_blk, Q_BLOCK, ATTN_HEAD_DIM), 3, 0)
    o_blocks = lax.map(lambda qb: diff_attend(qb, k_all, v_all, lam), q_blocks)
    o_lat = jnp.moveaxis(o_blocks, 0, 2).reshape(b_, ATTN_HEADS, n_tok, ATTN_V_DIM)

    def finish(o):
        n = o.shape[2]
        o = rmsnorm(o, subln_w, SUBLN_EPS) * (1 - lambda_init)
        return o.transpose(0, 2, 1, 3).reshape(b_, n, ATTN_HEADS * ATTN_V_DIM) @ w_o

    y_lat = finish(o_lat)
    y_ctx = finish(diff_attend(q_ctx, k_ctx, v_ctx, lam)) if need_ctx else None
    return y_lat, y_ctx


def centred_depthwise_conv(x, w, b):
    y = lax.conv_general_dilated(x, w[:, None, :], window_strides=(1,),
                                 padding=[(CONV_W // 2, CONV_W // 2)],
                                 dimension_numbers=('NWC', 'WIO', 'NWC'),
                                 feature_group_count=x.shape[-1])
    return y + b


def headwise_blockdiag(x, w):
    xb = x.reshape(*x.shape[:-1], MLSTM_INNER // QKV_BLOCK, QKV_BLOCK)
    return jnp.einsum('blgi,gio->blgo', xb, w).reshape(x.shape)


def mlstm_chunk_scan(q, k, v, i_pre, log_f, state):
    b_, nh, n_tok, dh = q.shape
    n_chunks = n_tok // CHUNK

    def chunks(a):
        return jnp.moveaxis(a.reshape(b_, nh, n_chunks, CHUNK, *a.shape[3:]), 2, 0)

    in_order = jnp.tril(jnp.ones((CHUNK, CHUNK), dtype=bool))

    def step(carry, inp):
        c_mat, n_vec, m_prev = carry
        qc, kc, vc, ic, fc = inp
        b_cum = jnp.cumsum(fc, axis=-1)
        log_d = jnp.where(in_order, b_cum[..., :, None] - b_cum[..., None, :] + ic[..., None, :], -jnp.inf)
        m_inter = b_cum + m_prev[..., None]
        m_t = jnp.maximum(m_inter, jnp.max(log_d, axis=-1))
        s = jnp.einsum('bhtd,bhsd->bhts', qc, kc) * jnp.exp(log_d - m_t[..., None])
        w_inter = jnp.exp(m_inter - m_t)
        num = jnp.einsum('bhts,bhsv->bhtv', s, vc) + w_inter[..., None] * jnp.einsum('bhvk,bhtk->bhtv', c_mat, qc)
        den = jnp.sum(s, axis=-1) + w_inter * jnp.einsum('bhk,bhtk->bht', n_vec, qc)
        h = num / jnp.maximum(jnp.abs(den), jnp.exp(-m_t))[..., None]
        b_tot = b_cum[..., -1]
        log_w = b_tot[..., None] - b_cum + ic
        m_new = jnp.maximum(b_tot + m_prev, jnp.max(log_w, axis=-1))
        w_s = jnp.exp(log_w - m_new[..., None])
        decay = jnp.exp(b_tot + m_prev - m_new)
        c_new = decay[..., None, None] * c_mat + jnp.einsum('bhs,bhsv,bhsk->bhvk', w_s, vc, kc)
        n_new = decay[..., None] * n_vec + jnp.einsum('bhs,bhsk->bhk', w_s, kc)
        return (c_new, n_new, m_new), h

    final, h_c = lax.scan(step, state, (chunks(q), chunks(k), chunks(v), chunks(i_pre), chunks(log_f)))
    return jnp.moveaxis(h_c, 0, 2).reshape(q.shape), final


def mlstm_bidirectional(cell, st_f, st_b):
    q, k, v, ig_f, lf_f, ig_b, lf_b = cell
    fl = lambda a: jnp.flip(a, axis=2)
    h_f, end_f = mlstm_chunk_scan(q, k, v, ig_f, lf_f, st_f)
    h_b, end_b = mlstm_chunk_scan(fl(q), fl(k), fl(v), fl(ig_b), fl(lf_b), st_b)
    return h_f + fl(h_b), end_f, end_b


def mlstm_features(u, w_up, conv_w, conv_b, w_qkv, w_gates, b_gates):
    b_, n_tok, _ = u.shape
    x_in, z = jnp.split(u @ w_up, 2, axis=-1)
    x_conv = jax.nn.silu(centred_depthwise_conv(x_in, conv_w, conv_b))
    q = headwise_blockdiag(x_conv, w_qkv[0])
    k = headwise_blockdiag(x_conv, w_qkv[1])
    v = headwise_blockdiag(x_in, w_qkv[2])
    gates = (jnp.concatenate([q, k, v], axis=-1) @ w_gates + b_gates).astype(jnp.float32)
    ig_f, fg_f, ig_b, fg_b = jnp.split(gates.transpose(0, 2, 1), 4, axis=1)

    def heads(t):
        return t.reshape(b_, n_tok, MLSTM_HEADS, MLSTM_HEAD_DIM).transpose(0, 2, 1, 3).astype(jnp.float32)

    cell = (heads(q), heads(k) * (MLSTM_HEAD_DIM ** -0.5), heads(v),
            ig_f, jax.nn.log_sigmoid(fg_f), ig_b, jax.nn.log_sigmoid(fg_b))
    return cell, x_conv, z


def mlstm_output(h, x_conv, z, skip, norm_w, w_down):
    b_, nh, n_tok, dh = h.shape
    mu = jnp.mean(h, axis=-1, keepdims=True)
    var = jnp.mean(jnp.square(h - mu), axis=-1, keepdims=True)
    hn = (h - mu) * lax.rsqrt(var + HEAD_LN_EPS) * norm_w.astype(jnp.float32).reshape(nh, 1, dh)
    hn = hn.transpose(0, 2, 1, 3).reshape(b_, n_tok, nh * dh).astype(x_conv.dtype)
    return ((hn + skip * x_conv) * jax.nn.silu(z)) @ w_down


def mlstm_mixer(u_lat, u_ctx, w_up, conv_w, conv_b, w_qkv, w_gates, b_gates, skip, norm_w, w_down, need_ctx):
    b_ = u_lat.shape[0]
    cell_ctx, xc_ctx, z_ctx = mlstm_features(u_ctx, w_up, conv_w, conv_b, w_qkv, w_gates, b_gates)
    cell_lat, xc_lat, z_lat = mlstm_features(u_lat, w_up, conv_w, conv_b, w_qkv, w_gates, b_gates)
    zero = (jnp.zeros((b_, MLSTM_HEADS, MLSTM_HEAD_DIM, MLSTM_HEAD_DIM), jnp.float32),
            jnp.zeros((b_, MLSTM_HEADS, MLSTM_HEAD_DIM), jnp.float32),
            jnp.zeros((b_, MLSTM_HEADS), jnp.float32))
    h_ctx, st_f, st_b = mlstm_bidirectional(cell_ctx, zero, zero)
    h_lat, _, _ = mlstm_bidirectional(cell_lat, st_f, st_b)
    y_lat = mlstm_output(h_lat, xc_lat, z_lat, skip, norm_w, w_down)
    y_ctx = mlstm_output(h_ctx, xc_ctx, z_ctx, skip, norm_w, w_down) if need_ctx else None
    return y_lat, y_ctx


def setup_inputs(seed: int = 0) -> dict:
    key = jax.random.key(seed)
    ks = jax.random.split(key, 24)

    def nrm(k, shape, scale):
        return jax.random.normal(k, shape, jnp.float32) * scale

    forget_base = jnp.linspace(3.0, 6.0, MLSTM_HEADS, dtype=jnp.float32)
    zeros_h = jnp.zeros((MLSTM_HEADS,), jnp.float32)
    gate_base = jnp.concatenate([zeros_h, forget_base, zeros_h, forget_base])
    return {
        'x': nrm(ks[0], (BATCH, SEQ, D_MODEL), 1.0),
        'c': nrm(ks[1], (BATCH, D_MODEL), 1.0),
        'ctx': nrm(ks[2], (BATCH, CTX_LEN, D_MODEL), 1.0),
        'c_ctx': nrm(ks[3], (D_MODEL,), 1.0),
        'w_mod': nrm(ks[4], (DEPTH, D_MODEL, N_MOD * D_MODEL), 0.5 * D_MODEL ** -0.5),
        'b_mod': nrm(ks[5], (DEPTH, N_MOD * D_MODEL), 0.02),
        'norm_w': 1.0 + nrm(ks[6], (DEPTH, 3, D_MODEL), 0.02),
        'ffn_w_in': nrm(ks[7], (DEPTH, 2, D_MODEL, 2 * D_FF), D_MODEL ** -0.5),
        'ffn_w_out': nrm(ks[8], (DEPTH, 2, D_FF, D_MODEL), D_FF ** -0.5),
        'attn_w_qkv': nrm(ks[9], (N_ATTN_LAYERS, D_MODEL, 3 * D_MODEL), D_MODEL ** -0.5),
        'attn_w_o': nrm(ks[10], (N_ATTN_LAYERS, D_MODEL, D_MODEL), D_MODEL ** -0.5),
        'attn_lambda': nrm(ks[11], (N_ATTN_LAYERS, 4, ATTN_HEAD_DIM), 0.1),
        'attn_subln_w': 1.0 + nrm(ks[12], (N_ATTN_LAYERS, ATTN_V_DIM), 0.02),
        'mlstm_w_up': nrm(ks[13], (N_MLSTM_LAYERS, D_MODEL, 2 * MLSTM_INNER), D_MODEL ** -0.5),
        'mlstm_conv_w': nrm(ks[14], (N_MLSTM_LAYERS, CONV_W, MLSTM_INNER), CONV_W ** -0.5),
        'mlstm_conv_b': nrm(ks[15], (N_MLSTM_LAYERS, MLSTM_INNER), 0.02),
        'mlstm_w_qkv': nrm(ks[16], (N_MLSTM_LAYERS, 3, MLSTM_INNER // QKV_BLOCK, QKV_BLOCK, QKV_BLOCK), QKV_BLOCK ** -0.5),
        'mlstm_w_gates': nrm(ks[17], (N_MLSTM_LAYERS, 3 * MLSTM_INNER, 4 * MLSTM_HEADS), 0.1 * (3 * MLSTM_INNER) ** -0.5),
        'mlstm_b_gates': gate_base + nrm(ks[18], (N_MLSTM_LAYERS, 4 * MLSTM_HEADS), 0.1),
        'mlstm_skip': 1.0 + nrm(ks[19], (N_MLSTM_LAYERS, MLSTM_INNER), 0.02),
        'mlstm_norm_w': 1.0 + nrm(ks[20], (N_MLSTM_LAYERS, MLSTM_INNER), 0.02),
        'mlstm_w_down': nrm(ks[21], (N_MLSTM_LAYERS, MLSTM_INNER, D_MODEL), MLSTM_INNER ** -0.5),
        'final_norm_w': 1.0 + nrm(ks[22], (D_MODEL,), 0.02),
    }


def reference(x, c, ctx, c_ctx, w_mod, b_mod, norm_w, ffn_w_in, ffn_w_out,
              attn_w_qkv, attn_w_o, attn_lambda, attn_subln_w,
              mlstm_w_up, mlstm_conv_w, mlstm_conv_b, mlstm_w_qkv, mlstm_w_gates, mlstm_b_gates,
              mlstm_skip, mlstm_norm_w, mlstm_w_down, final_norm_w):
    b_, n_tok, d = x.shape
    sc = jax.nn.silu(c)
    scc = jax.nn.silu(c_ctx)
    h_lat, h_ctx = x, ctx
    for i in range(DEPTH):
        last = i == DEPTH - 1
        mod_lat = (sc @ w_mod[i] + b_mod[i]).reshape(b_, 1, N_MOD, d)
        mod_ctx = (scc @ w_mod[i] + b_mod[i]).reshape(1, 1, N_MOD, d)
        h_lat = half_ffn(h_lat, norm_w[i, 0], mod_lat, 0, ffn_w_in[i, 0], ffn_w_out[i, 0])
        h_ctx = half_ffn(h_ctx, norm_w[i, 0], mod_ctx, 0, ffn_w_in[i, 0], ffn_w_out[i, 0])
        u_lat = adaln_input(h_lat, norm_w[i, 1], mod_lat, 1)
        u_ctx = adaln_input(h_ctx, norm_w[i, 1], mod_ctx, 1)
        j = i // N_MIXERS
        if i % N_MIXERS == 0:
            y_lat, y_ctx = diff_attention_mixer(u_lat, u_ctx, attn_w_qkv[j], attn_w_o[j], attn_lambda[j],
                                                attn_subln_w[j], i, not last)
        else:
            y_lat, y_ctx = mlstm_mixer(u_lat, u_ctx, mlstm_w_up[j], mlstm_conv_w[j], mlstm_conv_b[j],
                                       mlstm_w_qkv[j], mlstm_w_gates[j], mlstm_b_gates[j], mlstm_skip[j],
                                       mlstm_norm_w[j], mlstm_w_down[j], not last)
        h_lat = h_lat + mod_lat[:, :, 5] * y_lat
        h_lat = half_ffn(h_lat, norm_w[i, 2], mod_lat, 2, ffn_w_in[i, 1], ffn_w_out[i, 1])
        if not last:
            h_ctx = h_ctx + mod_ctx[:, :, 5] * y_ctx
            h_ctx = half_ffn(h_ctx, norm_w[i, 2], mod_ctx, 2, ffn_w_in[i, 1], ffn_w_out[i, 1])
    return rmsnorm(h_lat, final_norm_w)
```

```python
import math
from contextlib import ExitStack
import numpy as np
import concourse.bass as bass
import concourse.mybir as mybir
from concourse.bass_utils import run_bass_kernel_spmd

F32 = mybir.dt.float32
BF16 = mybir.dt.bfloat16
AF = mybir.ActivationFunctionType
ALU = mybir.AluOpType

D = 1024
KC = 8
DFF = 2816
NFF = 22
NMOD = 9
HD = 64
INNER = 2048
NH_M = 4
DH_M = 512
RMS_EPS = 1e-6
SUBLN_EPS = 1e-5
HEAD_LN_EPS = 1e-5
GRID_W = 64
ROPE_THETA = 10000.0
NEG = -30000.0


class Buf:
    def __init__(self, name, t):
        self.name = name
        self.t = t

    def __getitem__(self, idx):
        return self.t[idx]


class Sched:
    ENGS = ["pe", "act", "dve", "pool", "sp"]
    RING = 8

    def __init__(self, nc):
        self.nc = nc
        self.ops = {e: [] for e in self.ENGS}
        self.state = {}

    def _st(self, buf):
        return self.state.setdefault(buf.name, {})

    def _deps(self, buf, key, is_write):
        st = self._st(buf)
        keys = list(st.keys()) if key is None else [k for k in (key, None) if k in st]
        out = set()
        for k2 in keys:
            s = st[k2]
            if s["w"] is not None:
                out.add(s["w"])
            if is_write:
                for e, i in s["r"].items():
                    out.add((e, i))
        return out

    def op(self, eng, fn, reads=(), writes=(), dma=False, barrier=False):
        idx = len(self.ops[eng])
        deps = set()
        for (b, k) in reads:
            deps |= self._deps(b, k, False)
        for (b, k) in writes:
            deps |= self._deps(b, k, True)
        if eng == "pe":
            deps = {d for d in deps if d[0] != "pe"}
        deps.discard((eng, idx))
        self.ops[eng].append(dict(fn=fn, deps=deps, dma=dma, inc=False, barrier=barrier))
        for (b, k) in reads:
            st = self._st(b)
            s = st.setdefault(k, {"w": None, "r": {}})
            s["r"][eng] = idx
        for (b, k) in writes:
            st = self._st(b)
            if k is None:
                st.clear()
            st[k] = {"w": (eng, idx), "r": {}}

    def emit(self):
        nc = self.nc
        ops = self.ops
        for e in self.ENGS:
            for rec in ops[e]:
                for (e2, i2) in rec["deps"]:
                    ops[e2][i2]["inc"] = True
        with ExitStack() as es:
            cnt = {e: es.enter_context(nc.semaphore("c_" + e)) for e in ["pe", "act", "dve", "pool"]}
            ring = {e: [es.enter_context(nc.semaphore("r_%s%d" % (e, i))) for i in range(self.RING)]
                    for e in ["sp", "pool"]}
            for e in self.ENGS:
                c = 0
                uses = [0] * self.RING
                nd = 0
                for rec in ops[e]:
                    if rec["dma"]:
                        slot = nd % self.RING
                        nd += 1
                        uses[slot] += 1
                        rec["slot"] = slot
                        rec["tok"] = (("r", e, slot), 16 * uses[slot])
                        rec["pre"] = (("r", e, slot), 16 * (uses[slot] - 1))
                    else:
                        if rec["inc"]:
                            c += 1
                        rec["tok"] = (("c", e), c)
            semof = lambda k: cnt[k[1]] if k[0] == "c" else ring[k[1]][k[2]]
            final_tok = {}
            for e in ["sp", "pool"]:
                for rec in ops[e]:
                    if rec["dma"]:
                        final_tok[rec["tok"][0]] = rec["tok"][1]
            block = es.enter_context(nc.Block())

            def run(e, eng):
                waited = {}
                issued = {}
                for rec in ops[e]:
                    need = {}
                    if rec["barrier"]:
                        need.update(issued)
                    for (e2, i2) in rec["deps"]:
                        k, v = ops[e2][i2]["tok"]
                        need[k] = max(need.get(k, 0), v)
                    if rec["dma"] and rec["pre"][1] > 0:
                        k, v = rec["pre"]
                        need[k] = max(need.get(k, 0), v)
                    for k, v in need.items():
                        if waited.get(k, 0) < v:
                            eng.wait_ge(semof(k), v)
                            waited[k] = v
                    ins = rec["fn"](eng)
                    if rec["dma"]:
                        ins.then_inc(semof(rec["tok"][0]), 16)
                        issued[rec["tok"][0]] = rec["tok"][1]
                    elif rec["inc"]:
                        ins.then_inc(cnt[e], 1)
                if e == "sp":
                    for k, v in final_tok.items():
                        if waited.get(k, 0) < v:
                            eng.wait_ge(semof(k), v)

            @block.tensor
            def _(eng):
                run("pe", eng)

            @block.scalar
            def _(eng):
                run("act", eng)

            @block.vector
            def _(eng):
                run("dve", eng)

            @block.gpsimd
            def _(eng):
                run("pool", eng)

            @block.sync
            def _(eng):
                run("sp", eng)


class Cfg:
    def __init__(self, nctx=256, nlat=2048, layers=(0, 1, 2, 3), nseq=2, final=True, depth=4):
        self.nctx, self.nlat, self.layers, self.nseq, self.final, self.depth = nctx, nlat, tuple(layers), nseq, final, depth
        self.S = nctx + nlat
        self.tiles = []
        t = 0
        while t < nctx:
            n = min(512, nctx - t)
            self.tiles.append((t, n, True))
            t += n
        while t < self.S:
            n = min(512, self.S - t)
            self.tiles.append((t, n, False))
            t += n
        self.nchunk = self.S // 128
        self.cchunks = nctx // 128
        self.n_attn = (depth + 1) // 2
        self.n_ml = depth // 2


def build_program(cfg):
    nc = bass.Bass("TRN2", target_bir_lowering=False)
    S, NCTX, NLAT, NCH = cfg.S, cfg.nctx, cfg.nlat, cfg.nchunk
    L = cfg.depth
    NA, NM = max(cfg.n_attn, 1), max(cfg.n_ml, 1)
    dt = lambda name, shape, dtype=F32, kind="ExternalInput": nc.dram_tensor(name, list(shape), dtype, kind=kind).ap()
    xT = dt("xT", [cfg.nseq, D, S])
    outT = dt("outT", [cfg.nseq, D, NLAT if cfg.final else S], kind="ExternalOutput")
    cT = dt("cT", [D, 3])
    w_mod = dt("w_mod", [L, D, NMOD * D])
    bmodT = dt("bmodT", [L, 128, 72])
    normwT = dt("normwT", [128, L * 3 * 8])
    fnormT = dt("fnormT", [128, 8])
    w_in = dt("ffn_w_in", [L, 2, D, 2 * DFF])
    w_out = dt("ffn_w_out", [L, 2, DFF, D])
    a_wext = dt("a_wext", [NA, D, 8, 5, 128])
    a_wo = dt("a_wo", [NA, D, D])
    a_lam = dt("a_lam", [NA, 256])
    a_subln = dt("a_subln", [128, NA])
    ropeC = dt("ropeC", [128, NLAT])
    ropeS = dt("ropeS", [128, NLAT])
    m_wup = dt("m_wup", [NM, D, 2 * INNER])
    m_convT = dt("m_convT", [128, NM * 6 * 16])
    m_bd = dt("m_bd", [NM, 3, 16, 128, 128])
    m_bdT = dt("m_bdT", [NM, 3, 16, 128, 128])
    m_wg = dt("m_wg", [NM, 3 * INNER, 16])
    m_bg = dt("m_bg", [NM, 16])
    m_skipT = dt("m_skipT", [128, NM * 16])
    m_nwT = dt("m_nwT", [128, NM * 16])
    m_wdown = dt("m_wdown", [NM, INNER, D])
    cst = dt("cst", [5, 128, 128])
    xcs = dt("xcs", [16, 128, S], BF16, kind="Internal")
    vs = dt("vs", [4, 128, NCH, 512], BF16, kind="Internal")
    hfs = dt("hfs", [2, NCH, 128, 512], F32, kind="Internal")

    sch = Sched(nc)
    es = ExitStack()
    HFS, XCS, VS = Buf("d_hfs", None), Buf("d_xcs", None), Buf("d_vs", None)
    with es:
        def sbp(name, shape, dtype=F32):
            return Buf(name, es.enter_context(nc.sbuf_tensor(name, list(shape), dtype)))

        PS = [Buf("ps%d" % i, es.enter_context(nc.psum_tensor("ps%d" % i, [128, 512], F32))) for i in range(8)]
        ARENA_BYTES = 72 * 1024
        ARENA = es.enter_context(nc.sbuf_tensor("ARENA", [128, ARENA_BYTES // 2], BF16))
        ar = {"off": 0, "n": 0}

        def sb(name, shape, dtype=F32):
            esz = 4 if dtype == F32 else 2
            nel = 1
            for s_ in shape[1:]:
                nel *= s_
            nbytes = (nel * esz + 63) // 64 * 64
            off = ar["off"]
            assert off + nbytes <= ARENA_BYTES, (name, off, nbytes)
            ar["off"] = off + nbytes
            v = ARENA[0:shape[0], off // 2:(off + nel * esz) // 2]
            if dtype == F32:
                v = v.bitcast(F32)
            if len(shape) == 3:
                v = v.rearrange("p (a b) -> p a b", a=shape[1])
            elif len(shape) == 4:
                v = v.rearrange("p (a b c) -> p a b c", a=shape[1], b=shape[2])
            ar["n"] += 1
            return Buf("%s#%d" % (name, ar["n"]), v)

        def mm(out_b, out_ap, lhsT, rhs, start, stop, reads, okey=None):
            sch.op("pe", lambda e: e.matmul(out_ap, lhsT, rhs, start=start, stop=stop),
                   reads=reads, writes=[(out_b, okey)])

        def tr(out_b, out_ap, in_ap, ident_ap, reads, okey=None):
            sch.op("pe", lambda e: e.transpose(out_ap, in_ap, ident_ap), reads=reads, writes=[(out_b, okey)])

        def act(out_ap, in_ap, func, reads, writes, bias=None, scale=None):
            kw = {}
            if bias is not None:
                kw["bias"] = bias
            if scale is not None:
                kw["scale"] = scale
            sch.op("act", lambda e: e.activation(out=out_ap, in_=in_ap, func=func, **kw), reads=reads, writes=writes)

        def tt(eng, out_ap, in0, in1, op, reads, writes):
            sch.op(eng, lambda e: e.tensor_tensor(out=out_ap, in0=in0, in1=in1, op=op), reads=reads, writes=writes)

        def ts(eng, out_ap, in0, s1, s2, op0, op1, reads, writes):
            if op1 is None:
                sch.op(eng, lambda e: e.tensor_scalar(out=out_ap, in0=in0, scalar1=s1, scalar2=None, op0=op0),
                       reads=reads, writes=writes)
            else:
                sch.op(eng, lambda e: e.tensor_scalar(out=out_ap, in0=in0, scalar1=s1, scalar2=s2, op0=op0, op1=op1),
                       reads=reads, writes=writes)

        def stt(out_ap, in0, scalar, in1, op0, op1, reads, writes):
            sch.op("dve", lambda e: e.scalar_tensor_tensor(out=out_ap, in0=in0, scalar=scalar, in1=in1, op0=op0, op1=op1),
                   reads=reads, writes=writes)

        def cp(eng, out_ap, in_ap, reads, writes):
            if eng == "act":
                sch.op("act", lambda e: e.copy(out=out_ap, in_=in_ap), reads=reads, writes=writes)
            else:
                sch.op(eng, lambda e: e.tensor_copy(out=out_ap, in_=in_ap), reads=reads, writes=writes)

        def memset(eng, buf, ap, val):
            sch.op(eng, lambda e: e.memset(ap, val), writes=[(buf, None)])

        def dma(q, out_ap, in_ap, reads, writes, **kw):
            sch.op(q, lambda e: e.dma_start(out=out_ap, in_=in_ap, **kw), reads=reads, writes=writes, dma=True)

        H = sbp("H", [128, KC, S])
        U = sbp("U", [128, KC, S], BF16)
        CST = sbp("CST", [128, 5, 128])
        IDENT, TRIF, TRIB, MNEGF, MNEGB = (CST.t[:, i, :] for i in range(5))
        ONES32 = sbp("ONES32", [128, 128])
        ONESB = sbp("ONESB", [128, 128], BF16)
        M1024 = sbp("M1024", [128, 128], BF16)
        M128 = sbp("M128", [128, 128], BF16)
        ONEROW = sbp("ONEROW", [1, 128])
        MHALF = sbp("MHALF", [128, 8])
        MODT = sbp("MODT", [128, L, 72, 3])
        NWT = sbp("NWT", [128, L * 3 * 8])
        FNW = sbp("FNW", [128, 8])
        ACOEF = sbp("ACOEF", [128, L, 3, 3, 8])
        GCOEF = sbp("GCOEF", [128, L, 3, 3, 8])
        SQ = sbp("SQ", [128, KC, 256], BF16)
        LNT = sbp("LNT", [128, 512])
        TMPU = [sbp("TMPU%d" % i, [128, 256]) for i in range(2)]
        DUM = {e: sbp("DUM" + e, [128, 8]) for e in ["act", "dve", "pool", "sp"]}
        BAR = Buf("BAR", None)
        BAR2 = Buf("BAR2", None)

        def phase():
            for stage, bb in ((0, BAR), (1, BAR2)):
                for e in Sched.ENGS:
                    rd = [] if stage == 0 else [(BAR, k) for k in Sched.ENGS if k != e]
                    wr = [(bb, e)]
                    if e == "pe":
                        sch.op("pe", lambda en: en.matmul(PS[7].t[:, 0:2], ONESB.t[:, :], ONESB.t[:, 0:2], start=True, stop=True),
                               reads=rd + [(ONESB, None)], writes=wr + [(PS[7], None)], barrier=(stage == 0))
                    elif e == "sp":
                        sch.op("sp", lambda en: en.dma_start(out=DUM["sp"].t[0:1, 0:8], in_=cst[0, 0:1, 0:8]),
                               reads=rd, writes=wr + [(DUM["sp"], None)], dma=True, barrier=(stage == 0))
                    elif e == "act":
                        sch.op(e, lambda en: en.copy(out=DUM["act"].t[:], in_=ONES32.t[:, 0:8]), reads=rd + [(ONES32, None)],
                               writes=wr + [(DUM[e], None)], barrier=(stage == 0))
                    else:
                        sch.op(e, lambda en, e=e: en.memset(DUM[e].t[:], 0.0), reads=rd, writes=wr + [(DUM[e], None)],
                               barrier=(stage == 0))
            ar["off"] = 0

        dma("sp", CST.t[:], cst.rearrange("a p f -> p a f"), [], [(CST, None)])
        dma("sp", NWT.t[:], normwT, [], [(NWT, None)])
        dma("sp", FNW.t[:], fnormT, [], [(FNW, None)])
        memset("dve", ONES32, ONES32.t[:], 1.0)
        memset("dve", ONESB, ONESB.t[:], 1.0)
        memset("dve", M1024, M1024.t[:], 1.0 / 1024.0)
        memset("dve", M128, M128.t[:], 1.0 / 128.0)
        memset("dve", ONEROW, ONEROW.t[:], 1.0)
        memset("dve", MHALF, MHALF.t[:], -0.5)

        CT3 = sb("CT3", [128, KC, 3])
        SCT = sb("SCT", [128, KC, 3], BF16)
        dma("sp", CT3.t[:], cT.rearrange("(kc p) w -> p kc w", p=128), [], [(CT3, None)])
        act(SCT.t[:], CT3.t[:], AF.Silu, [(CT3, None)], [(SCT, None)])
        WM = [sb("WM%d" % i, [128, KC, 512], BF16) for i in range(2)]
        BMT = sb("BMT", [128, 72])
        for l in cfg.layers:
            dma("sp", BMT.t[:], bmodT[l], [], [(BMT, None)])
            for piece in range(18):
                wmb = WM[piece % 2]
                dma("pool", wmb.t[:], w_mod[l].rearrange("(kc p) n -> p kc n", p=128)[:, :, piece * 512:(piece + 1) * 512],
                    [], [(wmb, None)])
                for oc4 in range(4):
                    oc = piece * 4 + oc4
                    for kc in range(KC):
                        mm(PS[0], PS[0].t[:, oc * 3:oc * 3 + 3], wmb.t[:, kc, oc4 * 128:(oc4 + 1) * 128], SCT.t[:, kc, :],
                           kc == 0, kc == KC - 1, [(wmb, None), (SCT, None)])
            psv = PS[0].t[:, 0:216].rearrange("p (o w) -> p o w", w=3)
            for w in range(3):
                tt("dve", MODT.t[:, l, :, w], psv[:, :, w], BMT.t[:], ALU.add, [(PS[0], None), (BMT, None)], [(MODT, None)])
            for j in range(3):
                for w in range(3):
                    stt(ACOEF.t[:, l, j, w, :], MODT.t[:, l, (3 * j + 1) * 8:(3 * j + 2) * 8, w], 1.0,
                        NWT.t[:, (l * 3 + j) * 8:(l * 3 + j + 1) * 8], ALU.add, ALU.mult,
                        [(MODT, None), (NWT, None)], [(ACOEF, None)])
                    ts("dve", GCOEF.t[:, l, j, w, :], MODT.t[:, l, (3 * j + 2) * 8:(3 * j + 3) * 8, w],
                       (1.0 if j == 1 else 0.5), None, ALU.mult, None, [(MODT, None)], [(GCOEF, None)])

        def shiftc(l, j, w, c):
            return MODT.t[:, l, 3 * j * 8 + c, w:w + 1]

        def rstd_to_psum(src_sq_aps, mean_mat, psa, psb, n, eps, rd):
            k = len(src_sq_aps)
            for i, a in enumerate(src_sq_aps):
                mm(psa, psa.t[:, :n], mean_mat.t[:], a, i == 0, i == k - 1, rd + [(mean_mat, None)])
            act(LNT.t[:, :n], psa.t[:, :n], AF.Ln, [(psa, None)], [(LNT, None)], bias=eps)
            act(psb.t[:, :n], LNT.t[:, :n], AF.Exp, [(LNT, None)], [(psb, None)], scale=-0.5)

        def make_u_tile(l, j, seq, ti, out_fn=None):
            t0_, n_, isctx = cfg.tiles[ti]
            w = 2 if isctx else seq
            for t0 in range(t0_, t0_ + n_, 256):
                n = min(256, t0_ + n_ - t0)
                act(SQ.t[:, :, :n], H.t[:, :, t0:t0 + n], AF.Square, [(H, ti)], [(SQ, None)])
                rstd_to_psum([SQ.t[:, kc, :n] for kc in range(KC)], M1024, PS[6], PS[7], n, RMS_EPS, [(SQ, None)])
                for c in range(KC):
                    tb = TMPU[c % 2]
                    tt("dve", tb.t[:, :n], H.t[:, c, t0:t0 + n], PS[7].t[:, :n], ALU.mult, [(H, ti), (PS[7], None)], [(tb, None)])
                    if out_fn is None:
                        act(U.t[:, c, t0:t0 + n], tb.t[:, :n], AF.Identity, [(tb, None), (ACOEF, None), (MODT, None)], [(U, ti)],
                            bias=shiftc(l, j, w, c), scale=ACOEF.t[:, l, j, w, c:c + 1])
                    else:
                        out_fn(ti, t0, n, isctx, c, tb)

        def make_u(l, j, seq, out_fn=None):
            for ti in range(len(cfg.tiles)):
                make_u_tile(l, j, seq, ti, out_fn)

        groups = [(g * 4, min(4, NFF - g * 4)) for g in range((NFF + 3) // 4)]

        def ffn(l, f, j, seq, do_ctx=True):
            phase()
            WI = [sb("WI%d" % i, [128, KC, 2, 512], BF16) for i in range(2)]
            WO = [sb("WO%d" % i, [128, 4, D], BF16) for i in range(2)]
            SG = [sb("SG%d" % i, [128, 512]) for i in range(2)]
            AT = [sb("AT%d" % i, [128, 4, 512], BF16) for i in range(2)]
            tiles = [t for t in enumerate(cfg.tiles) if do_ctx or not t[1][2]]
            for gidx, (j0, ng) in enumerate(groups):
                wi, wo = WI[gidx % 2], WO[gidx % 2]
                src = w_in[l, f].rearrange("(kc p) (two n) -> p kc two n", p=128, two=2)[:, :, :, j0 * 128:(j0 + ng) * 128]
                for two in range(2):
                    dma("pool", wi.t[:, :, two, :ng * 128], src[:, :, two, :], [], [(wi, None)])
                dma("pool", wo.t[:, :ng, :], w_out[l, f].rearrange("(jj p) d -> p jj d", p=128)[:, j0:j0 + ng, :], [], [(wo, None)])

                def up(ti, t0, n, par):
                    at = AT[par]
                    for q in range(ng):
                        pg, pu = PS[q % 2], PS[2 + q % 2]
                        for kc in range(KC):
                            mm(pg, pg.t[:, :n], wi.t[:, kc, 0, q * 128:(q + 1) * 128], U.t[:, kc, t0:t0 + n], kc == 0, kc == KC - 1,
                               [(wi, None), (U, ti)])
                        for kc in range(KC):
                            mm(pu, pu.t[:, :n], wi.t[:, kc, 1, q * 128:(q + 1) * 128], U.t[:, kc, t0:t0 + n], kc == 0, kc == KC - 1,
                               [(wi, None), (U, ti)])
                        sg = SG[q % 2]
                        act(sg.t[:, :n], pg.t[:, :n], AF.Silu, [(pg, None)], [(sg, None)])
                        tt("dve", at.t[:, q, :n], sg.t[:, :n], pu.t[:, :n], ALU.mult, [(sg, None), (pu, None)], [(at, q)])

                def down(ti, t0, n, isctx, par):
                    at = AT[par]
                    w = 2 if isctx else seq
                    for c in range(KC):
                        py = PS[4 + c % 2]
                        for q in range(ng):
                            mm(py, py.t[:, :n], wo.t[:, q, c * 128:(c + 1) * 128], at.t[:, q, :n], q == 0, q == ng - 1,
                               [(wo, None), (at, q)])
                        stt(H.t[:, c, t0:t0 + n], py.t[:, :n], GCOEF.t[:, l, j, w, c:c + 1], H.t[:, c, t0:t0 + n], ALU.mult, ALU.add,
                            [(py, None), (GCOEF, None), (H, ti)], [(H, ti)])

                for i, (ti, (t0, n, isctx)) in enumerate(tiles):
                    if i == 0:
                        if gidx == 0:
                            make_u_tile(l, j, seq, ti)
                        up(ti, t0, n, 0)
                    if i + 1 < len(tiles):
                        ti2, (t02, n2, _) = tiles[i + 1]
                        if gidx == 0:
                            make_u_tile(l, j, seq, ti2)
                        up(ti2, t02, n2, (i + 1) % 2)
                    down(ti, t0, n, isctx, i % 2)

        def attention(l, seq, need_ctx):
            phase()
            W5 = sb("W5", [128, KC, 5, 128], BF16)
            WOH = [sb("WOH%d" % i, [128, D], BF16) for i in range(2)]
            QTM = [sb("QT%d" % i, [128, S], BF16) for i in range(2)]
            KT = sb("KT", [128, S], BF16)
            for m_ in range(2):
                memset("pool", QTM[m_], QTM[m_].t[(1 - m_) * 64:(2 - m_) * 64, :], 0.0)
            VT = sb("VT", [128, NCH, 128], BF16)
            COS = sb("COS", [128, NLAT])
            SINS = sb("SINS", [128, NLAT])
            LAMV = sb("LAMV", [128, 256])
            LAMP = sb("LAMP", [128, 128])
            LAMS = sb("LAMS", [128, 8])
            SUBW = sb("SUBW", [128, NA])
            R1 = [sb("R1_%d" % i, [128, 512]) for i in range(2)]
            PT = [sb("PT%d" % i, [128, 512], BF16) for i in range(3)]
            ON = [sb("ON%d" % i, [128, 512]) for i in range(2)]
            OF = sb("OF", [128, 512])
            OSQ = sb("OSQ", [128, 512], BF16)
            OHT = sb("OHT", [128, 512], BF16)
            dma("sp", COS.t[:], ropeC, [], [(COS, None)])
            dma("sp", SINS.t[:], ropeS, [], [(SINS, None)])
            dma("sp", SUBW.t[:], a_subln, [], [(SUBW, None)])
            ai = l // 2
            lam_init = 0.8 - 0.6 * math.exp(-0.3 * l)
            w = seq
            dma("sp", LAMV.t[:], a_lam[ai].partition_broadcast(128), [], [(LAMV, None)])
            tt("dve", LAMP.t[:, 0:64], LAMV.t[:, 0:64], LAMV.t[:, 64:128], ALU.mult, [(LAMV, None)], [(LAMP, None)])
            tt("dve", LAMP.t[:, 64:128], LAMV.t[:, 128:192], LAMV.t[:, 192:256], ALU.mult, [(LAMV, None)], [(LAMP, None)])
            for i in range(2):
                sch.op("dve", lambda e, i=i: e.tensor_reduce(out=LAMS.t[:, i:i + 1], in_=LAMP.t[:, i * 64:(i + 1) * 64],
                                                             axis=mybir.AxisListType.X, op=ALU.add),
                       reads=[(LAMP, None)], writes=[(LAMS, None)])
            act(LAMS.t[:, 4:6], LAMS.t[:, 0:2], AF.Exp, [(LAMS, None)], [(LAMS, None)])
            tt("dve", LAMS.t[:, 6:7], LAMS.t[:, 5:6], LAMS.t[:, 4:5], ALU.subtract, [(LAMS, None)], [(LAMS, None)])
            ts("dve", LAMS.t[:, 2:3], LAMS.t[:, 6:7], -lam_init, None, ALU.add, None, [(LAMS, None)], [(LAMS, None)])
            ts("dve", LAMS.t[:, 3:4], SUBW.t[:, ai:ai + 1], 1.0 - lam_init, None, ALU.mult, None, [(SUBW, None), (LAMS, None)], [(LAMS, None)])
            NEGLAM = LAMS.t[:, 2:3]
            SW = LAMS.t[:, 3:4]
            qtiles = [t for t in enumerate(cfg.tiles) if need_ctx or not t[1][2]]
            for h in range(8):
                w5 = W5
                woh = WOH[h % 2]
                dma("pool", w5.t[:], a_wext[ai].rearrange("(kc p) h f c -> p kc h f c", p=128)[:, :, h], [], [(w5, None)])
                dma("pool", woh.t[:], a_wo[ai, h * 128:(h + 1) * 128, :], [], [(woh, None)])
                for ti, (t0, n, isctx) in enumerate(cfg.tiles):
                    if h == 0:
                        make_u_tile(l, 1, seq, ti)
                    for (fi, dst) in ((0, None), (2, KT)):
                        pa, pb = PS[0], PS[1]
                        for kc in range(KC):
                            mm(pa, pa.t[:, :n], w5.t[:, kc, fi, :], U.t[:, kc, t0:t0 + n], kc == 0, kc == KC - 1, [(w5, None), (U, ti)])
                        if isctx:
                            if dst is None:
                                for m_ in range(2):
                                    rows = slice(m_ * 64, (m_ + 1) * 64)
                                    cp("act", QTM[m_].t[rows, t0:t0 + n], pa.t[rows, :n], [(pa, None)], [(QTM[m_], ti)])
                            else:
                                cp("act", dst.t[:, t0:t0 + n], pa.t[:, :n], [(pa, None)], [(dst, ti)])
                        else:
                            for kc in range(KC):
                                mm(pb, pb.t[:, :n], w5.t[:, kc, fi + 1, :], U.t[:, kc, t0:t0 + n], kc == 0, kc == KC - 1, [(w5, None), (U, ti)])
                            l0 = t0 - NCTX
                            tt("dve", R1[0].t[:, :n], pa.t[:, :n], COS.t[:, l0:l0 + n], ALU.mult, [(pa, None), (COS, None)], [(R1[0], None)])
                            tt("dve", R1[1].t[:, :n], pb.t[:, :n], SINS.t[:, l0:l0 + n], ALU.mult, [(pb, None), (SINS, None)], [(R1[1], None)])
                            if dst is None:
                                for m_ in range(2):
                                    rows = slice(m_ * 64, (m_ + 1) * 64)
                                    tt("pool", QTM[m_].t[rows, t0:t0 + n], R1[0].t[rows, :n], R1[1].t[rows, :n], ALU.add,
                                       [(R1[0], None), (R1[1], None)], [(QTM[m_], ti)])
                            else:
                                tt("pool", dst.t[:, t0:t0 + n], R1[0].t[:, :n], R1[1].t[:, :n], ALU.add, [(R1[0], None), (R1[1], None)], [(dst, ti)])
                    for s4 in range(0, n, 128):
                        tc = (t0 + s4) // 128
                        pv = PS[2]
                        for kc in range(KC):
                            mm(pv, pv.t[:, :128], U.t[:, kc, t0 + s4:t0 + s4 + 128], w5.t[:, kc, 4, :], kc == 0, kc == KC - 1, [(w5, None), (U, ti)])
                        cp("act", VT.t[:, tc, :], pv.t[:, :128], [(pv, None)], [(VT, tc)])
                pending = []

                def make_epilogue(ti, t0, n, isctx):
                    wsel = 2 if isctx else w

                    def part1():
                        tt("dve", ON[0].t[:, :n], ON[0].t[:, :n], R1[0].t[:, :n], ALU.mult, [(ON[0], None), (R1[0], None)], [(ON[0], None)])
                        stt(ON[1].t[:, :n], ON[1].t[:, :n], NEGLAM, R1[1].t[:, :n], ALU.mult, ALU.mult, [(ON[1], None), (R1[1], None), (LAMS, None)], [(ON[1], None)])
                        tt("dve", OF.t[:, :n], ON[0].t[:, :n], ON[1].t[:, :n], ALU.add, [(ON[0], None), (ON[1], None)], [(OF, None)])
                        act(OSQ.t[:, :n], OF.t[:, :n], AF.Square, [(OF, None)], [(OSQ, None)])
                        rstd_to_psum([OSQ.t[:, :n]], M128, PS[1], R1[0], n, SUBLN_EPS, [(OSQ, None)])
                        stt(OHT.t[:, :n], OF.t[:, :n], SW, R1[0].t[:, :n], ALU.mult, ALU.mult, [(OF, None), (LAMS, None), (R1[0], None)], [(OHT, None)])

                    def part2():
                        for c in range(KC):
                            py = PS[1 + c % 2]
                            mm(py, py.t[:, :n], woh.t[:, c * 128:(c + 1) * 128], OHT.t[:, :n], True, True, [(woh, None), (OHT, None)])
                            stt(H.t[:, c, t0:t0 + n], py.t[:, :n], GCOEF.t[:, l, 1, wsel, c:c + 1], H.t[:, c, t0:t0 + n], ALU.mult, ALU.add,
                                [(py, None), (GCOEF, None), (H, ti)], [(H, ti)])
                    return [part1, part2]

                for (ti, (t0, n, isctx)) in qtiles:
                    kchunks = list(range(cfg.cchunks)) if isctx else list(range(NCH))
                    for m in range(2):
                        po, psm = PS[3], PS[4]
                        nk = len(kchunks)

                        SB_ = [PS[5], PS[6], PS[7], PS[0]]

                        def s_mm(ki):
                            kc_ = kchunks[ki]
                            pS = SB_[ki % 4]
                            mm(pS, pS.t[:, :n], KT.t[:, kc_ * 128:(kc_ + 1) * 128], QTM[m].t[:, t0:t0 + n],
                               True, True, [(KT, None), (QTM[m], None)])

                        for k0 in range(min(3, nk)):
                            s_mm(k0)
                        for ki, kc_ in enumerate(kchunks):
                            pS = SB_[ki % 4]
                            pt = PT[ki % 3]
                            act(pt.t[:, :n], pS.t[:, :n], AF.Exp, [(pS, None)], [(pt, None)], scale=HD ** -0.5)
                            if ki + 3 < nk:
                                s_mm(ki + 3)
                            mm(po, po.t[:, :n], VT.t[:, kc_, :], pt.t[:, :n], ki == 0, ki == nk - 1, [(VT, None), (pt, None)])
                            mm(psm, psm.t[:, :n], ONESB.t[:], pt.t[:, :n], ki == 0, ki == nk - 1, [(ONESB, None), (pt, None)])
                            if m == 0 and pending and (ki == min(3, nk - 1) or ki == min(9, nk - 1)):
                                pending.pop(0)()
                        cp("dve", ON[m].t[:, :n], po.t[:, :n], [(po, None)], [(ON[m], None)])
                        act(LNT.t[:, :n], psm.t[:, :n], AF.Ln, [(psm, None)], [(LNT, None)])
                        act(R1[m].t[:, :n], LNT.t[:, :n], AF.Exp, [(LNT, None)], [(R1[m], None)], scale=-1.0)
                    while pending:
                        pending.pop(0)()
                    pending.extend(make_epilogue(ti, t0, n, isctx))
                while pending:
                    pending.pop(0)()

        def mlstm(l, seq, need_ctx):
            phase()
            mi = l // 2
            w = seq
            dscale = DH_M ** -0.5
            CONV = sb("CONV", [128, NM * 6 * 16])
            SKIPT = sb("SKIPT", [128, NM * 16])
            MNW = sb("MNW", [128, NM * 16])
            BD = sb("BD", [128, 3, 16, 128], BF16)
            WG = sb("WG", [128, 48, 16], BF16)
            WGC = sb("WGC", [128, 2, 16, 16], BF16)
            BGROW = sb("BGROW", [1, 16])
            GT = sb("GT", [128, NCH, 16])
            NL = sb("NL", [128, 2, NCH, 4])
            IG = sb("IG", [128, 2, NCH, 4])
            GB = sb("GB", [128, 2, NCH, 4])
            GIMB = sb("GIMB", [128, 2, NCH, 4])
            GW = sb("GW", [128, 2, NCH, 4])
            GEB = sb("GEB", [128, 2, NCH, 4])
            GDEC = sb("GDEC", [128, 2, NCH, 4])
            common_off = ar["off"]
            BDT = sb("BDT", [128, 3, 16, 128], BF16)
            dma("sp", CONV.t[:], m_convT, [], [(CONV, None)])
            dma("sp", SKIPT.t[:], m_skipT, [], [(SKIPT, None)])
            dma("sp", MNW.t[:], m_nwT, [], [(MNW, None)])
            cv = lambda tap, fc: CONV.t[:, (mi * 6 + tap) * 16 + fc:(mi * 6 + tap) * 16 + fc + 1]
            dma("pool", BD.t[:], m_bd[mi].rearrange("a c k o -> k a c o"), [], [(BD, None)])
            dma("pool", BDT.t[:], m_bdT[mi].rearrange("a c o k -> o a c k"), [], [(BDT, None)])
            dma("pool", WG.t[:], m_wg[mi].rearrange("(c p) g -> p c g", p=128), [], [(WG, None)])
            dma("sp", BGROW.t[:], m_bg[mi:mi + 1, :], [], [(BGROW, None)])
            for fc in range(16):
                pg = PS[0]
                mm(pg, pg.t[:, fc * 32:fc * 32 + 16], BDT.t[:, 0, fc, :], WG.t[:, fc, :], True, False, [(BDT, None), (WG, None)])
                mm(pg, pg.t[:, fc * 32:fc * 32 + 16], BDT.t[:, 1, fc, :], WG.t[:, 16 + fc, :], False, True, [(BDT, None), (WG, None)])
                mm(pg, pg.t[:, fc * 32 + 16:fc * 32 + 32], BDT.t[:, 2, fc, :], WG.t[:, 32 + fc, :], True, True, [(BDT, None), (WG, None)])
            pgv = PS[0].t[:, :].rearrange("p (c two g) -> p two c g", two=2, g=16)
            for two in range(2):
                cp("dve", WGC.t[:, two, :, :], pgv[:, two, :, :], [(PS[0], None)], [(WGC, None)])
            phase()
            ar["off"] = common_off
            WUP = [sb("WUP%d" % i, [128, KC, 128], BF16) for i in range(2)]
            XINB = [sb("XINB%d" % i, [128, S], BF16) for i in range(2)]
            XCB = [sb("XCB%d" % i, [128, S], BF16) for i in range(2)]
            DG = [sb("DG%d" % i, [128, 5, 128], BF16) for i in range(2)]
            VB = sb("VB", [128, NCH, 512], BF16)
            segs = [(0, NCTX), (NCTX, S)]
            for fc in range(16):
                head, fi = fc // 4, fc % 4
                wu, xinb, xcb, dg = WUP[fc % 2], XINB[fc % 2], XCB[fc % 2], DG[fc % 2]
                dma("pool", wu.t[:], m_wup[mi].rearrange("(kc p) n -> p kc n", p=128)[:, :, fc * 128:(fc + 1) * 128], [], [(wu, None)])
                for tap in range(5):
                    ts("pool" if tap % 2 else "dve", dg.t[:, tap, :], IDENT, cv(tap, fc), None, ALU.mult, None, [(CST, None), (CONV, None)], [(dg, None)])
                for ti, (t0, n, isctx) in enumerate(cfg.tiles):
                    if fc == 0:
                        make_u_tile(l, 1, seq, ti)
                    px = PS[ti % 2]
                    for kc in range(KC):
                        mm(px, px.t[:, :n], wu.t[:, kc, :], U.t[:, kc, t0:t0 + n], kc == 0, kc == KC - 1, [(wu, None), (U, ti)])
                    cp("act", xinb.t[:, t0:t0 + n], px.t[:, :n], [(px, None)], [(xinb, ti)])
                for ti, (t0, n, isctx) in enumerate(cfg.tiles):
                    s0, s1 = segs[0] if isctx else segs[1]
                    pc = PS[2 + ti % 2]
                    taps = [2, 0, 1, 3, 4]
                    for k_, tap in enumerate(taps):
                        d = tap - 2
                        a0, a1 = max(t0, s0 - d), min(t0 + n, s1 - d)
                        mm(pc, pc.t[:, a0 - t0:a1 - t0], dg.t[:, tap, :], xinb.t[:, a0 + d:a1 + d], k_ == 0, k_ == 4, [(dg, None), (xinb, None)])
                    act(xcb.t[:, t0:t0 + n], pc.t[:, :n], AF.Silu, [(pc, None), (CONV, None)], [(xcb, ti)], bias=cv(5, fc))
                dma("sp", xcs[fc], xcb.t[:], [(xcb, None)], [(XCS, fc)])
                for tc in range(NCH):
                    pv = PS[4 + tc % 2]
                    mm(pv, pv.t[:, :128], xinb.t[:, tc * 128:(tc + 1) * 128], BD.t[:, 2, fc, :], True, True, [(xinb, None), (BD, None)])
                    cp("dve", VB.t[:, tc, fi * 128:(fi + 1) * 128], pv.t[:, :128], [(pv, None)], [(VB, None)])
                    pgt = PS[6]
                    first = True
                    if fc == 0:
                        mm(pgt, pgt.t[:, tc * 16:(tc + 1) * 16], ONEROW.t[:], BGROW.t[:], True, False, [(ONEROW, None), (BGROW, None)])
                        first = False
                    mm(pgt, pgt.t[:, tc * 16:(tc + 1) * 16], xcb.t[:, tc * 128:(tc + 1) * 128], WGC.t[:, 0, fc, :], first, False, [(xcb, None), (WGC, None)])
                    mm(pgt, pgt.t[:, tc * 16:(tc + 1) * 16], xinb.t[:, tc * 128:(tc + 1) * 128], WGC.t[:, 1, fc, :], False, True, [(xinb, None), (WGC, None)])
                gtv = GT.t[:].rearrange("p c g -> p (c g)")
                if fc == 0:
                    cp("dve", gtv, PS[6].t[:, :NCH * 16], [(PS[6], None)], [(GT, None)])
                else:
                    tt("dve", gtv, gtv, PS[6].t[:, :NCH * 16], ALU.add, [(PS[6], None), (GT, None)], [(GT, None)])
                if fi == 3:
                    dma("sp", vs[head], VB.t[:], [(VB, None)], [(VS, head)])
            for d_ in range(2):
                cp("dve", IG.t[:, d_, :, :], GT.t[:, :, d_ * 8:d_ * 8 + 4], [(GT, None)], [(IG, None)])
                act(NL.t[:, d_, :, :], GT.t[:, :, d_ * 8 + 4:d_ * 8 + 8], AF.Exp, [(GT, None)], [(NL, None)], scale=-1.0)
            nlall = NL.t[:].rearrange("p a c h -> p (a c h)")
            act(nlall, nlall, AF.Ln, [(NL, None)], [(NL, None)], bias=1.0)
            for d_ in range(2):
                tri = TRIF if d_ == 0 else TRIB
                fl = lambda B_: B_.t[:, d_, :, :].rearrange("p c h -> p (c h)")
                pb_, pt_ = PS[0], PS[1]
                mm(pb_, pb_.t[:, :NCH * 4], tri, fl(NL), True, True, [(CST, None), (NL, None)])
                mm(pt_, pt_.t[:, :NCH * 4], ONES32.t[:], fl(NL), True, True, [(ONES32, None), (NL, None)])
                cp("dve", fl(GB), pb_.t[:, :NCH * 4], [(pb_, None)], [(GB, None)])
                tt("dve", fl(GIMB), fl(IG), fl(GB), ALU.add, [(IG, None), (GB, None)], [(GIMB, None)])
                tt("dve", fl(GW), fl(GIMB), pt_.t[:, :NCH * 4], ALU.subtract, [(GIMB, None), (pt_, None)], [(GW, None)])
                act(fl(GW), fl(GW), AF.Exp, [(GW, None)], [(GW, None)])
                ts("dve", fl(GW), fl(GW), dscale, None, ALU.mult, None, [(GW, None)], [(GW, None)])
                act(fl(GIMB), fl(GIMB), AF.Exp, [(GIMB, None)], [(GIMB, None)])
                act(fl(GEB), fl(GB), AF.Exp, [(GB, None)], [(GEB, None)], scale=-1.0)
                act(fl(GDEC), pt_.t[:, :NCH * 4], AF.Exp, [(pt_, None)], [(GDEC, None)], scale=-1.0)
            for head in range(NH_M):
                phase()
                ar["off"] = common_off
                ST = []
                for d_ in range(2):
                    st = dict(
                        XCC=[sb("XCC%d" % i, [128, 4, 128], BF16) for i in range(2)],
                        VC=[sb("VC%d" % i, [128, 512], BF16) for i in range(2)],
                        HB=[sb("HB%d" % i, [128, 512]) for i in range(2)],
                        CTS=sb("CTS", [128, 4, 512]), CTB=sb("CTB", [128, 4, 512], BF16),
                        NV=sb("NV", [128, 4]), NVB=sb("NVB", [128, 4], BF16),
                        QC=sb("QC", [128, 4, 128], BF16), KCB=sb("KCB", [128, 4, 128], BF16), KW=sb("KW", [128, 512], BF16),
                        SD=sb("SD", [128, 128], BF16), DEN=sb("DEN", [128, 8]),
                        X=[PS[4 * d_], PS[4 * d_ + 1]], Y=PS[4 * d_ + 2], N=PS[4 * d_ + 3])
                    ST.append(st)

                def scan(d_):
                    st = ST[d_]
                    order = list(range(NCH)) if d_ == 0 else (list(range(cfg.cchunks - 1, -1, -1)) + list(range(NCH - 1, cfg.cchunks - 1, -1)))
                    mask = TRIF if d_ == 0 else TRIB
                    CTS, CTB, NV, NVB, QC, KCB, KW, SD, DEN = (st[k] for k in ("CTS", "CTB", "NV", "NVB", "QC", "KCB", "KW", "SD", "DEN"))
                    X0, X1, Y, N_ = st["X"][0], st["X"][1], st["Y"], st["N"]
                    memset("pool", CTS, CTS.t[:], 0.0)
                    memset("pool", CTB, CTB.t[:], 0.0)
                    memset("pool", NV, NV.t[:], 0.0)
                    memset("pool", NVB, NVB.t[:], 0.0)
                    yield
                    for oi, tc in enumerate(order):
                        col = lambda B_: B_.t[:, d_, tc, head:head + 1]
                        xcc, vc, hb = st["XCC"][oi % 2], st["VC"][oi % 2], st["HB"][oi % 2]
                        dma("sp", xcc.t[:], xcs[head * 4:(head + 1) * 4, :, tc * 128:(tc + 1) * 128].rearrange("c p s -> p c s"),
                            [(XCS, None)], [(xcc, None)])
                        dma("sp", vc.t[:], vs[head, :, tc, :], [(VS, head)], [(vc, None)])
                        for i in range(4):
                            mm(X0, X0.t[:, i * 128:(i + 1) * 128], BD.t[:, 0, head * 4 + i, :], xcc.t[:, i, :], True, True, [(BD, None), (xcc, None)])
                        cp("act", QC.t[:].rearrange("p a b -> p (a b)"), X0.t[:, :], [(X0, None)], [(QC, None)])
                        for i in range(4):
                            mm(X1, X1.t[:, i * 128:(i + 1) * 128], BD.t[:, 1, head * 4 + i, :], xcc.t[:, i, :], True, True, [(BD, None), (xcc, None)])
                        ts("dve", KCB.t[:].rearrange("p a b -> p (a b)"), X1.t[:, :], dscale, None, ALU.mult, None, [(X1, None)], [(KCB, None)])
                        yield
                        for i in range(4):
                            mm(X0, X0.t[:, i * 128:(i + 1) * 128], xcc.t[:, i, :], BD.t[:, 1, head * 4 + i, :], True, True, [(BD, None), (xcc, None)])
                        act(KW.t[:], X0.t[:, :], AF.Identity, [(X0, None), (GW, None)], [(KW, None)], scale=col(GW))
                        for i in range(4):
                            mm(Y, Y.t[:, :128], KCB.t[:, i, :], QC.t[:, i, :], i == 0, i == 3, [(KCB, None), (QC, None)])
                        stt(SD.t[:], Y.t[:, :128], col(GIMB), mask, ALU.mult, ALU.mult, [(Y, None), (GIMB, None), (CST, None)], [(SD, None)])
                        yield
                        mm(N_, N_.t[:, :], SD.t[:], vc.t[:], True, False, [(SD, None), (vc, None)])
                        for i in range(4):
                            mm(N_, N_.t[:, :], QC.t[:, i, :], CTB.t[:, i, :], False, i == 3, [(QC, None), (CTB, None)])
                        mm(Y, Y.t[:, 256:257], SD.t[:], ONESB.t[:, 0:1], True, False, [(SD, None), (ONESB, None)])
                        for i in range(4):
                            mm(Y, Y.t[:, 256:257], QC.t[:, i, :], NVB.t[:, i:i + 1], False, i == 3, [(QC, None), (NVB, None)])
                        act(DEN.t[:, 0:1], Y.t[:, 256:257], AF.Abs, [(Y, None), (GEB, None)], [(DEN, None)], scale=col(GEB))
                        ts("dve", DEN.t[:, 1:2], DEN.t[:, 0:1], 1.0, None, ALU.max, None, [(DEN, None)], [(DEN, None)])
                        sch.op("dve", lambda e: e.reciprocal(out=DEN.t[:, 2:3], in_=DEN.t[:, 1:2]), reads=[(DEN, None)], writes=[(DEN, None)])
                        tt("dve", DEN.t[:, 3:4], DEN.t[:, 2:3], col(GEB), ALU.mult, [(DEN, None), (GEB, None)], [(DEN, None)])
                        act(hb.t[:], N_.t[:, :], AF.Identity, [(N_, None), (DEN, None)], [(hb, None)], scale=DEN.t[:, 3:4])
                        dma("sp", hfs[d_, tc], hb.t[:], [(hb, None)], [(HFS, (d_, tc))])
                        yield
                        for i in range(4):
                            pu = st["X"][(i + 1) % 2]
                            mm(pu, pu.t[:, :], KW.t[:, i * 128:(i + 1) * 128], vc.t[:], True, True, [(KW, None), (vc, None)])
                            stt(CTS.t[:, i, :], CTS.t[:, i, :], col(GDEC), pu.t[:, :], ALU.mult, ALU.add, [(CTS, i), (GDEC, None), (pu, None)], [(CTS, i)])
                            cp("act", CTB.t[:, i, :], CTS.t[:, i, :], [(CTS, i)], [(CTB, i)])
                            if i % 2 == 1:
                                yield
                        for i in range(4):
                            mm(Y, Y.t[:, 264 + 2 * i:265 + 2 * i], KW.t[:, i * 128:(i + 1) * 128], ONESB.t[:, 0:1], True, True, [(KW, None), (ONESB, None)])
                        pnv = Y.t[:, 264:272].rearrange("p (a b) -> p a b", b=2)[:, :, 0]
                        stt(NV.t[:], NV.t[:], col(GDEC), pnv, ALU.mult, ALU.add, [(NV, None), (GDEC, None), (Y, None)], [(NV, None)])
                        cp("dve", NVB.t[:], NV.t[:], [(NV, None)], [(NVB, None)])
                        yield

                gens = [scan(0), scan(1)]
                alive = [True, True]
                while any(alive):
                    for gi, g in enumerate(gens):
                        if alive[gi]:
                            try:
                                next(g)
                            except StopIteration:
                                alive[gi] = False
                phase()
                ar["off"] = common_off
                WUZ = sb("WUZ", [128, KC, 512], BF16)
                WD = sb("WD", [128, 4, D], BF16)
                XCO = [sb("XCO%d" % i, [128, 4, 256], BF16) for i in range(2)]
                HF = [sb("HF%d" % i, [128, 2, 512]) for i in range(2)]
                HBW = [sb("HBW%d" % i, [128, 2, 512]) for i in range(2)]
                BNS_ = [sb("BNS%d" % i, [128, 2, 8]) for i in range(2)]
                RS_ = [sb("RS%d" % i, [128, 4]) for i in range(2)]
                TN_ = [sb("TN%d" % i, [128, 4, 256]) for i in range(2)]
                SZ = sb("SZ", [128, 4, 256])
                MT = sb("MT", [128, 4, 256], BF16)
                dma("pool", WUZ.t[:], m_wup[mi].rearrange("(kc p) n -> p kc n", p=128)[:, :, INNER + head * 512:INNER + (head + 1) * 512],
                    [], [(WUZ, None)])
                dma("pool", WD.t[:], m_wdown[mi].rearrange("(c p) d -> p c d", p=128)[:, head * 4:(head + 1) * 4, :], [], [(WD, None)])
                ogroups = []
                for (c0, c1) in ((0, cfg.cchunks), (cfg.cchunks, NCH)):
                    if c0 == 0 and not need_ctx:
                        continue
                    tc = c0
                    while tc < c1:
                        ogroups.append(list(range(tc, min(tc + 2, c1))))
                        tc += 2
                for gi_, grp in enumerate(ogroups):
                    par = gi_ % 2
                    ng = len(grp)
                    T = 128 * ng
                    g0 = grp[0]
                    tok = slice(g0 * 128, g0 * 128 + T)
                    xco, hf, hbw, BNS, RS, TN = XCO[par], HF[par], HBW[par], BNS_[par], RS_[par], TN_[par]
                    isctx = g0 < cfg.cchunks
                    ti = [k for k, (a, n_, _) in enumerate(cfg.tiles) if a <= g0 * 128 < a + n_][0]
                    wsel = 2 if isctx else w
                    dma("sp", xco.t[:, :, :T], xcs[head * 4:(head + 1) * 4, :, g0 * 128:g0 * 128 + T].rearrange("c p s -> p c s"), [(XCS, None)], [(xco, None)])
                    dma("sp", hf.t[:, :ng, :], hfs[0, g0:g0 + ng].rearrange("c p f -> p c f"), [(HFS, None)], [(hf, None)])
                    dma("sp", hbw.t[:, :ng, :], hfs[1, g0:g0 + ng].rearrange("c p f -> p c f"), [(HFS, None)], [(hbw, None)])
                    tt("pool", hf.t[:, :ng, :], hf.t[:, :ng, :], hbw.t[:, :ng, :], ALU.add, [(hf, None), (hbw, None)], [(hf, None)])
                    tt("pool", TN.t[:, :, :T], xco.t[:, :, :T], SKIPT.t[:, mi * 16 + head * 4:mi * 16 + head * 4 + 4].unsqueeze(2).to_broadcast([128, 4, T]), ALU.mult,
                       [(xco, None), (SKIPT, None)], [(TN, None)])
                    pzb = lambda i: PS[i // 2].t[:, (i % 2) * 256:(i % 2) * 256 + T]
                    phb = lambda i: PS[2 + i // 2].t[:, (i % 2) * 256:(i % 2) * 256 + T]
                    for i in range(4):
                        for kc in range(KC):
                            mm(PS[i // 2], pzb(i), WUZ.t[:, kc, i * 128:(i + 1) * 128], U.t[:, kc, tok], kc == 0, kc == KC - 1, [(WUZ, None), (U, ti)])
                    for j in range(ng):
                        sch.op("dve", lambda e, hf=hf, BNS=BNS, j=j: e.bn_stats(out=BNS.t[:, j, 0:6], in_=hf.t[:, j, :]), reads=[(hf, None)], writes=[(BNS, None)])
                        sch.op("dve", lambda e, BNS=BNS, j=j: e.bn_aggr(out=BNS.t[:, j, 6:8], in_=BNS.t[:, j, 0:6]), reads=[(BNS, None)], writes=[(BNS, None)])
                    act(RS.t[:, 0:ng], BNS.t[:, 0:ng, 7], AF.Ln, [(BNS, None)], [(RS, None)], bias=HEAD_LN_EPS)
                    act(RS.t[:, 0:ng], RS.t[:, 0:ng], AF.Exp, [(RS, None)], [(RS, None)], scale=-0.5)
                    for j in range(ng):
                        ts("dve", hf.t[:, j, :], hf.t[:, j, :], BNS.t[:, j, 6:7], RS.t[:, j:j + 1], ALU.subtract, ALU.mult, [(hf, None), (BNS, None), (RS, None)], [(hf, None)])
                    for b in range(2):
                        pzv = PS[b].t[:, 0:512].rearrange("p (a t) -> p a t", a=2)[:, :, :T]
                        szv = SZ.t[:, 2 * b:2 * b + 2, :T]
                        act(szv, pzv, AF.Exp, [(PS[b], None)], [(SZ, b)], scale=-1.0)
                        act(szv, szv, AF.Ln, [(SZ, b)], [(SZ, b)], bias=1.0)
                        act(szv, szv, AF.Exp, [(SZ, b)], [(SZ, b)], scale=-1.0)
                        stt(szv, pzv, 1.0, szv, ALU.mult, ALU.mult, [(PS[b], None), (SZ, b)], [(SZ, b)])
                    for j in range(ng):
                        for i in range(4):
                            tr(PS[2 + i // 2], PS[2 + i // 2].t[:, (i % 2) * 256 + j * 128:(i % 2) * 256 + (j + 1) * 128],
                               hf.t[:, j, i * 128:(i + 1) * 128], IDENT, [(hf, None), (CST, None)])
                    for i in range(4):
                        fcg = mi * 16 + head * 4 + i
                        stt(TN.t[:, i, :T], phb(i), MNW.t[:, fcg:fcg + 1], TN.t[:, i, :T], ALU.mult, ALU.add,
                            [(PS[2 + i // 2], None), (MNW, None), (TN, None)], [(TN, None)])
                    tt("dve", MT.t[:, :, :T], TN.t[:, :, :T], SZ.t[:, :, :T], ALU.mult, [(TN, None), (SZ, None)], [(MT, None)])
                    for c in range(KC):
                        py = PS[4 + c % 4]
                        for i in range(4):
                            mm(py, py.t[:, :T], WD.t[:, i, c * 128:(c + 1) * 128], MT.t[:, i, :T], i == 0, i == 3, [(WD, None), (MT, None)])
                        stt(H.t[:, c, tok], py.t[:, :T], GCOEF.t[:, l, 1, wsel, c:c + 1], H.t[:, c, tok], ALU.mult, ALU.add,
                            [(py, None), (GCOEF, None), (H, ti)], [(H, ti)])

        for seq in range(cfg.nseq):
            phase()
            for ti, (t0, n, isctx) in enumerate(cfg.tiles):
                dma("sp", H.t[:, :, t0:t0 + n], xT[seq].rearrange("(kc p) s -> p kc s", p=128)[:, :, t0:t0 + n], [], [(H, ti)])
            for l in cfg.layers:
                last = (l == cfg.depth - 1)
                ffn(l, 0, 0, seq, True)
                if l % 2 == 0:
                    attention(l, seq, not last)
                else:
                    mlstm(l, seq, not last)
                ffn(l, 1, 2, seq, not last)
            phase()
            OUTB = [sb("OUTB%d" % i, [128, 256]) for i in range(2)]
            cnt = [0]

            def fin(ti, t0, n, isctx, c, tb):
                if cfg.final and isctx:
                    return
                ob = OUTB[cnt[0] % 2]
                cnt[0] += 1
                if cfg.final:
                    ts("dve", ob.t[:, :n], tb.t[:, :n], FNW.t[:, c:c + 1], None, ALU.mult, None, [(tb, None), (FNW, None)], [(ob, None)])
                    dma("sp", outT[seq, c * 128:(c + 1) * 128, t0 - NCTX:t0 - NCTX + n], ob.t[:, :n], [(ob, None)], [])
            if cfg.final:
                make_u(0, 0, seq, out_fn=fin)
            else:
                for ti, (t0, n, isctx) in enumerate(cfg.tiles):
                    dma("sp", outT[seq].rearrange("(kc p) s -> p kc s", p=128)[:, :, t0:t0 + n], H.t[:, :, t0:t0 + n], [(H, ti)], [])
        sch.emit()
    return nc


def _rope_tables(nlat):
    rows = nlat // GRID_W
    row_pos = np.repeat(np.arange(rows, dtype=np.float32), GRID_W)
    col_pos = np.tile(np.arange(GRID_W, dtype=np.float32), rows)
    half = HD // 2
    inv_freq = (1.0 / (ROPE_THETA ** (np.arange(0, half, 2, dtype=np.float32) / half))).astype(np.float32)
    C = np.zeros((128, nlat), np.float32)
    Sg = np.zeros((128, nlat), np.float32)
    for p in range(128):
        d = p % 64
        axis, r, fq = d // 32, (d % 32) // 16, d % 16
        ang = (row_pos if axis == 0 else col_pos) * inv_freq[fq]
        C[p] = np.cos(ang)
        Sg[p] = np.sin(ang) * (-1.0 if r == 0 else 1.0)
    return C, Sg


def _consts():
    s = np.arange(128)[:, None]
    t = np.arange(128)[None, :]
    ident = np.eye(128, dtype=np.float32)
    triF = (s <= t).astype(np.float32)
    triB = (s >= t).astype(np.float32)
    mF = np.where(s <= t, 0.0, -NEG).astype(np.float32)
    mB = np.where(s >= t, 0.0, -NEG).astype(np.float32)
    return np.stack([ident, triF, triB, mF, mB]).astype(np.float32)


def _prep_shared(inp, cfg):
    L = cfg.depth
    f = lambda a: np.ascontiguousarray(np.asarray(a, dtype=np.float32))
    sh = {}
    sh["w_mod"] = f(inp["w_mod"])
    sh["bmodT"] = f(np.asarray(inp["b_mod"]).reshape(L, 72, 128).transpose(0, 2, 1))
    sh["normwT"] = f(np.asarray(inp["norm_w"]).reshape(L, 3, 8, 128).transpose(3, 0, 1, 2).reshape(128, L * 24))
    sh["fnormT"] = f(np.asarray(inp["final_norm_w"]).reshape(8, 128).T)
    sh["ffn_w_in"] = f(inp["ffn_w_in"])
    sh["ffn_w_out"] = f(inp["ffn_w_out"])
    NA, NM = max(cfg.n_attn, 1), max(cfg.n_ml, 1)
    wqkv = np.asarray(inp["attn_w_qkv"], dtype=np.float32)
    na = wqkv.shape[0]
    p = np.arange(128)
    d = p % 64
    partner = np.where((d % 32) < 16, p + 16, p - 16)
    ext = np.zeros((NA, D, 8, 5, 128), np.float32)
    for a in range(min(na, NA)):
        q = wqkv[a][:, 0:D].reshape(D, 8, 128)
        k = wqkv[a][:, D:2 * D].reshape(D, 8, 128)
        v = wqkv[a][:, 2 * D:3 * D].reshape(D, 8, 128)
        ext[a, :, :, 0] = q
        ext[a, :, :, 1] = q[:, :, partner]
        ext[a, :, :, 2] = k
        ext[a, :, :, 3] = k[:, :, partner]
        ext[a, :, :, 4] = v
    sh["a_wext"] = ext
    pad = lambda a, n: f(np.concatenate([np.asarray(a, np.float32)] + [np.asarray(a, np.float32)[:1]] * (n - np.asarray(a).shape[0]), 0)) if np.asarray(a).shape[0] < n else f(np.asarray(a)[:n])
    sh["a_wo"] = pad(inp["attn_w_o"], NA)
    sh["a_lam"] = pad(np.asarray(inp["attn_lambda"]).reshape(-1, 256), NA)
    sh["a_subln"] = f(pad(inp["attn_subln_w"], NA).T)
    C, Sg = _rope_tables(cfg.nlat)
    sh["ropeC"], sh["ropeS"] = C, Sg
    sh["m_wup"] = pad(inp["mlstm_w_up"], NM)
    cw = pad(inp["mlstm_conv_w"], NM)
    cb = pad(inp["mlstm_conv_b"], NM)
    cc = np.concatenate([cw, cb[:, None, :]], 1)
    sh["m_convT"] = f(cc.reshape(NM, 6, 16, 128).transpose(3, 0, 1, 2).reshape(128, NM * 96))
    wq = pad(inp["mlstm_w_qkv"], NM)
    bd = np.zeros((NM, 3, 16, 128, 128), np.float32)
    for c in range(16):
        for g in range(32):
            bd[:, :, c, g * 4:(g + 1) * 4, g * 4:(g + 1) * 4] = wq[:, :, c * 32 + g]
    sh["m_bd"] = bd
    sh["m_bdT"] = f(bd.transpose(0, 1, 2, 4, 3))
    sh["m_wg"] = pad(inp["mlstm_w_gates"], NM)
    sh["m_bg"] = pad(inp["mlstm_b_gates"], NM)
    sh["m_skipT"] = f(pad(inp["mlstm_skip"], NM).reshape(NM, 16, 128).transpose(2, 0, 1).reshape(128, NM * 16))
    sh["m_nwT"] = f(pad(inp["mlstm_norm_w"], NM).reshape(NM, 16, 128).transpose(2, 0, 1).reshape(128, NM * 16))
    sh["m_wdown"] = pad(inp["mlstm_w_down"], NM)
    sh["cst"] = _consts()
    return sh


def run_cfg(inp, cfg, n_cores=8):
    x = np.asarray(inp["x"], np.float32)
    ctx = np.asarray(inp["ctx"], np.float32)
    c = np.asarray(inp["c"], np.float32)
    c_ctx = np.asarray(inp["c_ctx"], np.float32)
    B = x.shape[0]
    assert B == n_cores * cfg.nseq
    sh = _prep_shared(inp, cfg)
    nc = build_program(cfg)
    in_maps = []
    for core in range(n_cores):
        bs = [core * cfg.nseq + i for i in range(cfg.nseq)]
        xt = np.stack([np.concatenate([ctx[b], x[b]], 0).T for b in bs]).astype(np.float32)
        cols = [c[bs[i]] if i < cfg.nseq else c[bs[0]] for i in range(2)] + [c_ctx]
        m = dict(sh)
        m["xT"] = np.ascontiguousarray(xt)
        m["cT"] = np.ascontiguousarray(np.stack(cols, 1).astype(np.float32))
        in_maps.append(m)
    res = run_bass_kernel_spmd(nc, in_maps, core_ids=list(range(n_cores)))
    outs = []
    for core in range(n_cores):
        o = np.asarray(res.results[core]["outT"])
        for i in range(cfg.nseq):
            outs.append(o[i].T)
    return np.ascontiguousarray(np.stack(outs).astype(np.float32))


def kernel(**inputs):
    cfg = Cfg()
    return run_cfg(inputs, cfg)
```

```python
import math
from contextlib import ExitStack
import numpy as np
import concourse.bass as bass
import concourse.mybir as mybir
from concourse.bass_utils import run_bass_kernel_spmd

F32 = mybir.dt.float32
BF16 = mybir.dt.bfloat16
AF = mybir.ActivationFunctionType
ALU = mybir.AluOpType

D = 1024
KC = 8
DFF = 2816
NFF = 22
NMOD = 9
HD = 64
INNER = 2048
NH_M = 4
DH_M = 512
RMS_EPS = 1e-6
SUBLN_EPS = 1e-5
HEAD_LN_EPS = 1e-5
GRID_W = 64
ROPE_THETA = 10000.0
NEG = -30000.0


class Buf:
    def __init__(self, name, t):
        self.name = name
        self.t = t

    def __getitem__(self, idx):
        return self.t[idx]


class Sched:
    ENGS = ["pe", "act", "dve", "pool", "sp"]
    RING = 8

    def __init__(self, nc):
        self.nc = nc
        self.ops = {e: [] for e in self.ENGS}
        self.state = {}

    def _st(self, buf):
        return self.state.setdefault(buf.name, {})

    def _deps(self, buf, key, is_write):
        st = self._st(buf)
        keys = list(st.keys()) if key is None else [k for k in (key, None) if k in st]
        out = set()
        for k2 in keys:
            s = st[k2]
            if s["w"] is not None:
                out.add(s["w"])
            if is_write:
                for e, i in s["r"].items():
                    out.add((e, i))
        return out

    def op(self, eng, fn, reads=(), writes=(), dma=False, barrier=False):
        idx = len(self.ops[eng])
        deps = set()
        for (b, k) in reads:
            deps |= self._deps(b, k, False)
        for (b, k) in writes:
            deps |= self._deps(b, k, True)
        if eng == "pe":
            deps = {d for d in deps if d[0] != "pe"}
        deps.discard((eng, idx))
        self.ops[eng].append(dict(fn=fn, deps=deps, dma=dma, inc=False, barrier=barrier))
        for (b, k) in reads:
            st = self._st(b)
            s = st.setdefault(k, {"w": None, "r": {}})
            s["r"][eng] = idx
        for (b, k) in writes:
            st = self._st(b)
            if k is None:
                st.clear()
            st[k] = {"w": (eng, idx), "r": {}}

    def emit(self):
        nc = self.nc
        ops = self.ops
        for e in self.ENGS:
            for rec in ops[e]:
                for (e2, i2) in rec["deps"]:
                    ops[e2][i2]["inc"] = True
        with ExitStack() as es:
            cnt = {e: es.enter_context(nc.semaphore("c_" + e)) for e in ["pe", "act", "dve", "pool"]}
            ring = {e: [es.enter_context(nc.semaphore("r_%s%d" % (e, i))) for i in range(self.RING)]
                    for e in ["sp", "pool"]}
            for e in self.ENGS:
                c = 0
                uses = [0] * self.RING
                nd = 0
                for rec in ops[e]:
                    if rec["dma"]:
                        slot = nd % self.RING
                        nd += 1
                        uses[slot] += 1
                        rec["slot"] = slot
                        rec["tok"] = (("r", e, slot), 16 * uses[slot])
                        rec["pre"] = (("r", e, slot), 16 * (uses[slot] - 1))
                    else:
                        if rec["inc"]:
                            c += 1
                        rec["tok"] = (("c", e), c)
            semof = lambda k: cnt[k[1]] if k[0] == "c" else ring[k[1]][k[2]]
            final_tok = {}
            for e in ["sp", "pool"]:
                for rec in ops[e]:
                    if rec["dma"]:
                        final_tok[rec["tok"][0]] = rec["tok"][1]
            block = es.enter_context(nc.Block())

            def run(e, eng):
                waited = {}
                issued = {}
                for rec in ops[e]:
                    need = {}
                    if rec["barrier"]:
                        need.update(issued)
                    for (e2, i2) in rec["deps"]:
                        k, v = ops[e2][i2]["tok"]
                        need[k] = max(need.get(k, 0), v)
                    if rec["dma"] and rec["pre"][1] > 0:
                        k, v = rec["pre"]
                        need[k] = max(need.get(k, 0), v)
                    for k, v in need.items():
                        if waited.get(k, 0) < v:
                            eng.wait_ge(semof(k), v)
                            waited[k] = v
                    ins = rec["fn"](eng)
                    if rec["dma"]:
                        ins.then_inc(semof(rec["tok"][0]), 16)
                        issued[rec["tok"][0]] = rec["tok"][1]
                    elif rec["inc"]:
                        ins.then_inc(cnt[e], 1)
                if e == "sp":
                    for k, v in final_tok.items():
                        if waited.get(k, 0) < v:
                            eng.wait_ge(semof(k), v)

            @block.tensor
            def _(eng):
                run("pe", eng)

            @block.scalar
            def _(eng):
                run("act", eng)

            @block.vector
            def _(eng):
                run("dve", eng)

            @block.gpsimd
            def _(eng):
                run("pool", eng)

            @block.sync
            def _(eng):
                run("sp", eng)


class Cfg:
    def __init__(self, nctx=256, nlat=2048, layers=(0, 1, 2, 3), nseq=2, final=True, depth=4):
        self.nctx, self.nlat, self.layers, self.nseq, self.final, self.depth = nctx, nlat, tuple(layers), nseq, final, depth
        self.S = nctx + nlat
        self.tiles = []
        t = 0
        while t < nctx:
            n = min(512, nctx - t)
            self.tiles.append((t, n, True))
            t += n
        while t < self.S:
            n = min(512, self.S - t)
            self.tiles.append((t, n, False))
            t += n
        self.nchunk = self.S // 128
        self.cchunks = nctx // 128
        self.n_attn = (depth + 1) // 2
        self.n_ml = depth // 2


def build_program(cfg):
    nc = bass.Bass("TRN2", target_bir_lowering=False)
    S, NCTX, NLAT, NCH = cfg.S, cfg.nctx, cfg.nlat, cfg.nchunk
    L = cfg.depth
    NA, NM = max(cfg.n_attn, 1), max(cfg.n_ml, 1)
    dt = lambda name, shape, dtype=F32, kind="ExternalInput": nc.dram_tensor(name, list(shape), dtype, kind=kind).ap()
    xT = dt("xT", [cfg.nseq, D, S])
    outT = dt("outT", [cfg.nseq, D, NLAT if cfg.final else S], kind="ExternalOutput")
    cT = dt("cT", [D, 3])
    w_mod = dt("w_mod", [L, D, NMOD * D])
    bmodT = dt("bmodT", [L, 128, 72])
    normwT = dt("normwT", [128, L * 3 * 8])
    fnormT = dt("fnormT", [128, 8])
    w_in = dt("ffn_w_in", [L, 2, D, 2 * DFF])
    w_out = dt("ffn_w_out", [L, 2, DFF, D])
    a_wext = dt("a_wext", [NA, D, 8, 5, 128])
    a_wo = dt("a_wo", [NA, D, D])
    a_lam = dt("a_lam", [NA, 256])
    a_subln = dt("a_subln", [128, NA])
    ropeC = dt("ropeC", [128, NLAT])
    ropeS = dt("ropeS", [128, NLAT])
    m_wup = dt("m_wup", [NM, D, 2 * INNER])
    m_convT = dt("m_convT", [128, NM * 6 * 16])
    m_bd = dt("m_bd", [NM, 3, 16, 128, 128])
    m_bdT = dt("m_bdT", [NM, 3, 16, 128, 128])
    m_wg = dt("m_wg", [NM, 3 * INNER, 16])
    m_bg = dt("m_bg", [NM, 16])
    m_skipT = dt("m_skipT", [128, NM * 16])
    m_nwT = dt("m_nwT", [128, NM * 16])
    m_wdown = dt("m_wdown", [NM, INNER, D])
    cst = dt("cst", [5, 128, 128])
    xcs = dt("xcs", [16, 128, S], BF16, kind="Internal")
    vs = dt("vs", [4, 128, NCH, 512], BF16, kind="Internal")
    hfs = dt("hfs", [2, NCH, 128, 512], F32, kind="Internal")

    sch = Sched(nc)
    es = ExitStack()
    HFS, XCS, VS = Buf("d_hfs", None), Buf("d_xcs", None), Buf("d_vs", None)
    with es:
        def sbp(name, shape, dtype=F32):
            return Buf(name, es.enter_context(nc.sbuf_tensor(name, list(shape), dtype)))

        PS = [Buf("ps%d" % i, es.enter_context(nc.psum_tensor("ps%d" % i, [128, 512], F32))) for i in range(8)]
        ARENA_BYTES = 72 * 1024
        ARENA = es.enter_context(nc.sbuf_tensor("ARENA", [128, ARENA_BYTES // 2], BF16))
        ar = {"off": 0, "n": 0}

        def sb(name, shape, dtype=F32):
            esz = 4 if dtype == F32 else 2
            nel = 1
            for s_ in shape[1:]:
                nel *= s_
            nbytes = (nel * esz + 63) // 64 * 64
            off = ar["off"]
            assert off + nbytes <= ARENA_BYTES, (name, off, nbytes)
            ar["off"] = off + nbytes
            v = ARENA[0:shape[0], off // 2:(off + nel * esz) // 2]
            if dtype == F32:
                v = v.bitcast(F32)
            if len(shape) == 3:
                v = v.rearrange("p (a b) -> p a b", a=shape[1])
            elif len(shape) == 4:
                v = v.rearrange("p (a b c) -> p a b c", a=shape[1], b=shape[2])
            ar["n"] += 1
            return Buf("%s#%d" % (name, ar["n"]), v)

        def mm(out_b, out_ap, lhsT, rhs, start, stop, reads, okey=None):
            sch.op("pe", lambda e: e.matmul(out_ap, lhsT, rhs, start=start, stop=stop),
                   reads=reads, writes=[(out_b, okey)])

        def tr(out_b, out_ap, in_ap, ident_ap, reads, okey=None):
            sch.op("pe", lambda e: e.transpose(out_ap, in_ap, ident_ap), reads=reads, writes=[(out_b, okey)])

        def act(out_ap, in_ap, func, reads, writes, bias=None, scale=None):
            kw = {}
            if bias is not None:
                kw["bias"] = bias
            if scale is not None:
                kw["scale"] = scale
            sch.op("act", lambda e: e.activation(out=out_ap, in_=in_ap, func=func, **kw), reads=reads, writes=writes)

        def tt(eng, out_ap, in0, in1, op, reads, writes):
            sch.op(eng, lambda e: e.tensor_tensor(out=out_ap, in0=in0, in1=in1, op=op), reads=reads, writes=writes)

        def ts(eng, out_ap, in0, s1, s2, op0, op1, reads, writes):
            if op1 is None:
                sch.op(eng, lambda e: e.tensor_scalar(out=out_ap, in0=in0, scalar1=s1, scalar2=None, op0=op0),
                       reads=reads, writes=writes)
            else:
                sch.op(eng, lambda e: e.tensor_scalar(out=out_ap, in0=in0, scalar1=s1, scalar2=s2, op0=op0, op1=op1),
                       reads=reads, writes=writes)

        def stt(out_ap, in0, scalar, in1, op0, op1, reads, writes):
            sch.op("dve", lambda e: e.scalar_tensor_tensor(out=out_ap, in0=in0, scalar=scalar, in1=in1, op0=op0, op1=op1),
                   reads=reads, writes=writes)

        def cp(eng, out_ap, in_ap, reads, writes):
            if eng == "act":
                sch.op("act", lambda e: e.copy(out=out_ap, in_=in_ap), reads=reads, writes=writes)
            else:
                sch.op(eng, lambda e: e.tensor_copy(out=out_ap, in_=in_ap), reads=reads, writes=writes)

        def memset(eng, buf, ap, val):
            sch.op(eng, lambda e: e.memset(ap, val), writes=[(buf, None)])

        def dma(q, out_ap, in_ap, reads, writes, **kw):
            sch.op(q, lambda e: e.dma_start(out=out_ap, in_=in_ap, **kw), reads=reads, writes=writes, dma=True)

        H = sbp("H", [128, KC, S])
        U = sbp("U", [128, KC, S], BF16)
        CST = sbp("CST", [128, 5, 128])
        IDENT, TRIF, TRIB, MNEGF, MNEGB = (CST.t[:, i, :] for i in range(5))
        ONES32 = sbp("ONES32", [128, 128])
        ONESB = sbp("ONESB", [128, 128], BF16)
        M1024 = sbp("M1024", [128, 128], BF16)
        M128 = sbp("M128", [128, 128], BF16)
        ONEROW = sbp("ONEROW", [1, 128])
        MHALF = sbp("MHALF", [128, 8])
        MODT = sbp("MODT", [128, L, 72, 3])
        NWT = sbp("NWT", [128, L * 3 * 8])
        FNW = sbp("FNW", [128, 8])
        ACOEF = sbp("ACOEF", [128, L, 3, 3, 8])
        GCOEF = sbp("GCOEF", [128, L, 3, 3, 8])
        SQ = sbp("SQ", [128, KC, 256], BF16)
        LNT = sbp("LNT", [128, 512])
        TMPU = [sbp("TMPU%d" % i, [128, 256]) for i in range(2)]
        DUM = {e: sbp("DUM" + e, [128, 8]) for e in ["act", "dve", "pool", "sp"]}
        BAR = Buf("BAR", None)
        BAR2 = Buf("BAR2", None)

        def phase():
            for stage, bb in ((0, BAR), (1, BAR2)):
                for e in Sched.ENGS:
                    rd = [] if stage == 0 else [(BAR, k) for k in Sched.ENGS if k != e]
                    wr = [(bb, e)]
                    if e == "pe":
                        sch.op("pe", lambda en: en.matmul(PS[7].t[:, 0:2], ONESB.t[:, :], ONESB.t[:, 0:2], start=True, stop=True),
                               reads=rd + [(ONESB, None)], writes=wr + [(PS[7], None)], barrier=(stage == 0))
                    elif e == "sp":
                        sch.op("sp", lambda en: en.dma_start(out=DUM["sp"].t[0:1, 0:8], in_=cst[0, 0:1, 0:8]),
                               reads=rd, writes=wr + [(DUM["sp"], None)], dma=True, barrier=(stage == 0))
                    elif e == "act":
                        sch.op(e, lambda en: en.copy(out=DUM["act"].t[:], in_=ONES32.t[:, 0:8]), reads=rd + [(ONES32, None)],
                               writes=wr + [(DUM[e], None)], barrier=(stage == 0))
                    else:
                        sch.op(e, lambda en, e=e: en.memset(DUM[e].t[:], 0.0), reads=rd, writes=wr + [(DUM[e], None)],
                               barrier=(stage == 0))
            ar["off"] = 0

        dma("sp", CST.t[:], cst.rearrange("a p f -> p a f"), [], [(CST, None)])
        dma("sp", NWT.t[:], normwT, [], [(NWT, None)])
        dma("sp", FNW.t[:], fnormT, [], [(FNW, None)])
        memset("dve", ONES32, ONES32.t[:], 1.0)
        memset("dve", ONESB, ONESB.t[:], 1.0)
        memset("dve", M1024, M1024.t[:], 1.0 / 1024.0)
        memset("dve", M128, M128.t[:], 1.0 / 128.0)
        memset("dve", ONEROW, ONEROW.t[:], 1.0)
        memset("dve", MHALF, MHALF.t[:], -0.5)

        CT3 = sb("CT3", [128, KC, 3])
        SCT = sb("SCT", [128, KC, 3], BF16)
        dma("sp", CT3.t[:], cT.rearrange("(kc p) w -> p kc w", p=128), [], [(CT3, None)])
        act(SCT.t[:], CT3.t[:], AF.Silu, [(CT3, None)], [(SCT, None)])
        WM = [sb("WM%d" % i, [128, KC, 512], BF16) for i in range(2)]
        BMT = sb("BMT", [128, 72])
        for l in cfg.layers:
            dma("sp", BMT.t[:], bmodT[l], [], [(BMT, None)])
            for piece in range(18):
                wmb = WM[piece % 2]
                dma("pool", wmb.t[:], w_mod[l].rearrange("(kc p) n -> p kc n", p=128)[:, :, piece * 512:(piece + 1) * 512],
                    [], [(wmb, None)])
                for oc4 in range(4):
                    oc = piece * 4 + oc4
                    for kc in range(KC):
                        mm(PS[0], PS[0].t[:, oc * 3:oc * 3 + 3], wmb.t[:, kc, oc4 * 128:(oc4 + 1) * 128], SCT.t[:, kc, :],
                           kc == 0, kc == KC - 1, [(wmb, None), (SCT, None)])
            psv = PS[0].t[:, 0:216].rearrange("p (o w) -> p o w", w=3)
            for w in range(3):
                tt("dve", MODT.t[:, l, :, w], psv[:, :, w], BMT.t[:], ALU.add, [(PS[0], None), (BMT, None)], [(MODT, None)])
            for j in range(3):
                for w in range(3):
                    stt(ACOEF.t[:, l, j, w, :], MODT.t[:, l, (3 * j + 1) * 8:(3 * j + 2) * 8, w], 1.0,
                        NWT.t[:, (l * 3 + j) * 8:(l * 3 + j + 1) * 8], ALU.add, ALU.mult,
                        [(MODT, None), (NWT, None)], [(ACOEF, None)])
                    ts("dve", GCOEF.t[:, l, j, w, :], MODT.t[:, l, (3 * j + 2) * 8:(3 * j + 3) * 8, w],
                       (1.0 if j == 1 else 0.5), None, ALU.mult, None, [(MODT, None)], [(GCOEF, None)])

        def shiftc(l, j, w, c):
            return MODT.t[:, l, 3 * j * 8 + c, w:w + 1]

        def rstd_to_psum(src_sq_aps, mean_mat, psa, psb, n, eps, rd):
            k = len(src_sq_aps)
            for i, a in enumerate(src_sq_aps):
                mm(psa, psa.t[:, :n], mean_mat.t[:], a, i == 0, i == k - 1, rd + [(mean_mat, None)])
            act(LNT.t[:, :n], psa.t[:, :n], AF.Ln, [(psa, None)], [(LNT, None)], bias=eps)
            act(psb.t[:, :n], LNT.t[:, :n], AF.Exp, [(LNT, None)], [(psb, None)], scale=-0.5)

        def make_u_tile(l, j, seq, ti, out_fn=None):
            t0_, n_, isctx = cfg.tiles[ti]
            w = 2 if isctx else seq
            for t0 in range(t0_, t0_ + n_, 256):
                n = min(256, t0_ + n_ - t0)
                act(SQ.t[:, :, :n], H.t[:, :, t0:t0 + n], AF.Square, [(H, ti)], [(SQ, None)])
                rstd_to_psum([SQ.t[:, kc, :n] for kc in range(KC)], M1024, PS[6], PS[7], n, RMS_EPS, [(SQ, None)])
                for c in range(KC):
                    tb = TMPU[c % 2]
                    tt("dve", tb.t[:, :n], H.t[:, c, t0:t0 + n], PS[7].t[:, :n], ALU.mult, [(H, ti), (PS[7], None)], [(tb, None)])
                    if out_fn is None:
                        act(U.t[:, c, t0:t0 + n], tb.t[:, :n], AF.Identity, [(tb, None), (ACOEF, None), (MODT, None)], [(U, ti)],
                            bias=shiftc(l, j, w, c), scale=ACOEF.t[:, l, j, w, c:c + 1])
                    else:
                        out_fn(ti, t0, n, isctx, c, tb)

        def make_u(l, j, seq, out_fn=None):
            for ti in range(len(cfg.tiles)):
                make_u_tile(l, j, seq, ti, out_fn)

        groups = [(g * 4, min(4, NFF - g * 4)) for g in range((NFF + 3) // 4)]

        def ffn(l, f, j, seq, do_ctx=True):
            phase()
            WI = [sb("WI%d" % i, [128, KC, 2, 512], BF16) for i in range(2)]
            WO = [sb("WO%d" % i, [128, 4, D], BF16) for i in range(2)]
            SG = [sb("SG%d" % i, [128, 512]) for i in range(2)]
            AT = [sb("AT%d" % i, [128, 4, 512], BF16) for i in range(2)]
            tiles = [t for t in enumerate(cfg.tiles) if do_ctx or not t[1][2]]
            for gidx, (j0, ng) in enumerate(groups):
                wi, wo = WI[gidx % 2], WO[gidx % 2]
                src = w_in[l, f].rearrange("(kc p) (two n) -> p kc two n", p=128, two=2)[:, :, :, j0 * 128:(j0 + ng) * 128]
                for two in range(2):
                    dma("pool", wi.t[:, :, two, :ng * 128], src[:, :, two, :], [], [(wi, None)])
                dma("pool", wo.t[:, :ng, :], w_out[l, f].rearrange("(jj p) d -> p jj d", p=128)[:, j0:j0 + ng, :], [], [(wo, None)])

                def up(ti, t0, n, par):
                    at = AT[par]
                    for q in range(ng):
                        pg, pu = PS[q % 2], PS[2 + q % 2]
                        for kc in range(KC):
                            mm(pg, pg.t[:, :n], wi.t[:, kc, 0, q * 128:(q + 1) * 128], U.t[:, kc, t0:t0 + n], kc == 0, kc == KC - 1,
                               [(wi, None), (U, ti)])
                        for kc in range(KC):
                            mm(pu, pu.t[:, :n], wi.t[:, kc, 1, q * 128:(q + 1) * 128], U.t[:, kc, t0:t0 + n], kc == 0, kc == KC - 1,
                               [(wi, None), (U, ti)])
                        sg = SG[q % 2]
                        act(sg.t[:, :n], pg.t[:, :n], AF.Silu, [(pg, None)], [(sg, None)])
                        tt("dve", at.t[:, q, :n], sg.t[:, :n], pu.t[:, :n], ALU.mult, [(sg, None), (pu, None)], [(at, q)])

                def down(ti, t0, n, isctx, par):
                    at = AT[par]
                    w = 2 if isctx else seq
                    for c in range(KC):
                        py = PS[4 + c % 2]
                        for q in range(ng):
                            mm(py, py.t[:, :n], wo.t[:, q, c * 128:(c + 1) * 128], at.t[:, q, :n], q == 0, q == ng - 1,
                               [(wo, None), (at, q)])
                        stt(H.t[:, c, t0:t0 + n], py.t[:, :n], GCOEF.t[:, l, j, w, c:c + 1], H.t[:, c, t0:t0 + n], ALU.mult, ALU.add,
                            [(py, None), (GCOEF, None), (H, ti)], [(H, ti)])

                if gidx == 0:
                    for k_ in range(min(2, len(tiles))):
                        make_u_tile(l, j, seq, tiles[k_][0])
                for i, (ti, (t0, n, isctx)) in enumerate(tiles):
                    if gidx == 0 and i + 2 < len(tiles):
                        make_u_tile(l, j, seq, tiles[i + 2][0])
                    if i == 0:
                        up(ti, t0, n, 0)
                    if i + 1 < len(tiles):
                        ti2, (t02, n2, _) = tiles[i + 1]
                        up(ti2, t02, n2, (i + 1) % 2)
                    down(ti, t0, n, isctx, i % 2)

        def attention(l, seq, need_ctx):
            phase()
            W5 = sb("W5", [128, KC, 5, 128], BF16)
            WOH = [sb("WOH%d" % i, [128, D], BF16) for i in range(2)]
            QTM = [sb("QT%d" % i, [128, S], BF16) for i in range(2)]
            KT = sb("KT", [128, S], BF16)
            for m_ in range(2):
                memset("pool", QTM[m_], QTM[m_].t[(1 - m_) * 64:(2 - m_) * 64, :], 0.0)
            VT = sb("VT", [128, NCH, 128], BF16)
            COS = sb("COS", [128, NLAT])
            SINS = sb("SINS", [128, NLAT])
            LAMV = sb("LAMV", [128, 256])
            LAMP = sb("LAMP", [128, 128])
            LAMS = sb("LAMS", [128, 8])
            SUBW = sb("SUBW", [128, NA])
            R1 = [sb("R1_%d" % i, [128, 512]) for i in range(2)]
            PT = [sb("PT%d" % i, [128, 512], BF16) for i in range(3)]
            ON = [sb("ON%d" % i, [128, 512]) for i in range(2)]
            OF = sb("OF", [128, 512])
            OSQ = sb("OSQ", [128, 512], BF16)
            OHT = sb("OHT", [128, 512], BF16)
            dma("sp", COS.t[:], ropeC, [], [(COS, None)])
            dma("sp", SINS.t[:], ropeS, [], [(SINS, None)])
            dma("sp", SUBW.t[:], a_subln, [], [(SUBW, None)])
            ai = l // 2
            lam_init = 0.8 - 0.6 * math.exp(-0.3 * l)
            w = seq
            dma("sp", LAMV.t[:], a_lam[ai].partition_broadcast(128), [], [(LAMV, None)])
            tt("dve", LAMP.t[:, 0:64], LAMV.t[:, 0:64], LAMV.t[:, 64:128], ALU.mult, [(LAMV, None)], [(LAMP, None)])
            tt("dve", LAMP.t[:, 64:128], LAMV.t[:, 128:192], LAMV.t[:, 192:256], ALU.mult, [(LAMV, None)], [(LAMP, None)])
            for i in range(2):
                sch.op("dve", lambda e, i=i: e.tensor_reduce(out=LAMS.t[:, i:i + 1], in_=LAMP.t[:, i * 64:(i + 1) * 64],
                                                             axis=mybir.AxisListType.X, op=ALU.add),
                       reads=[(LAMP, None)], writes=[(LAMS, None)])
            act(LAMS.t[:, 4:6], LAMS.t[:, 0:2], AF.Exp, [(LAMS, None)], [(LAMS, None)])
            tt("dve", LAMS.t[:, 6:7], LAMS.t[:, 5:6], LAMS.t[:, 4:5], ALU.subtract, [(LAMS, None)], [(LAMS, None)])
            ts("dve", LAMS.t[:, 2:3], LAMS.t[:, 6:7], -lam_init, None, ALU.add, None, [(LAMS, None)], [(LAMS, None)])
            ts("dve", LAMS.t[:, 3:4], SUBW.t[:, ai:ai + 1], 1.0 - lam_init, None, ALU.mult, None, [(SUBW, None), (LAMS, None)], [(LAMS, None)])
            NEGLAM = LAMS.t[:, 2:3]
            SW = LAMS.t[:, 3:4]
            qtiles = [t for t in enumerate(cfg.tiles) if need_ctx or not t[1][2]]
            for h in range(8):
                w5 = W5
                woh = WOH[h % 2]
                dma("pool", w5.t[:], a_wext[ai].rearrange("(kc p) h f c -> p kc h f c", p=128)[:, :, h], [], [(w5, None)])
                dma("pool", woh.t[:], a_wo[ai, h * 128:(h + 1) * 128, :], [], [(woh, None)])
                for ti, (t0, n, isctx) in enumerate(cfg.tiles):
                    if h == 0:
                        if ti == 0:
                            make_u_tile(l, 1, seq, 0)
                        if ti + 1 < len(cfg.tiles):
                            make_u_tile(l, 1, seq, ti + 1)
                    for (fi, dst) in ((0, None), (2, KT)):
                        pa, pb = PS[0], PS[1]
                        for kc in range(KC):
                            mm(pa, pa.t[:, :n], w5.t[:, kc, fi, :], U.t[:, kc, t0:t0 + n], kc == 0, kc == KC - 1, [(w5, None), (U, ti)])
                        if isctx:
                            if dst is None:
                                for m_ in range(2):
                                    rows = slice(m_ * 64, (m_ + 1) * 64)
                                    cp("act", QTM[m_].t[rows, t0:t0 + n], pa.t[rows, :n], [(pa, None)], [(QTM[m_], ti)])
                            else:
                                cp("act", dst.t[:, t0:t0 + n], pa.t[:, :n], [(pa, None)], [(dst, ti)])
                        else:
                            for kc in range(KC):
                                mm(pb, pb.t[:, :n], w5.t[:, kc, fi + 1, :], U.t[:, kc, t0:t0 + n], kc == 0, kc == KC - 1, [(w5, None), (U, ti)])
                            l0 = t0 - NCTX
                            tt("dve", R1[0].t[:, :n], pa.t[:, :n], COS.t[:, l0:l0 + n], ALU.mult, [(pa, None), (COS, None)], [(R1[0], None)])
                            tt("dve", R1[1].t[:, :n], pb.t[:, :n], SINS.t[:, l0:l0 + n], ALU.mult, [(pb, None), (SINS, None)], [(R1[1], None)])
                            if dst is None:
                                for m_ in range(2):
                                    rows = slice(m_ * 64, (m_ + 1) * 64)
                                    tt("pool", QTM[m_].t[rows, t0:t0 + n], R1[0].t[rows, :n], R1[1].t[rows, :n], ALU.add,
                                       [(R1[0], None), (R1[1], None)], [(QTM[m_], ti)])
                            else:
                                tt("pool", dst.t[:, t0:t0 + n], R1[0].t[:, :n], R1[1].t[:, :n], ALU.add, [(R1[0], None), (R1[1], None)], [(dst, ti)])
                    for s4 in range(0, n, 128):
                        tc = (t0 + s4) // 128
                        pv = PS[2]
                        for kc in range(KC):
                            mm(pv, pv.t[:, :128], U.t[:, kc, t0 + s4:t0 + s4 + 128], w5.t[:, kc, 4, :], kc == 0, kc == KC - 1, [(w5, None), (U, ti)])
                        cp("act", VT.t[:, tc, :], pv.t[:, :128], [(pv, None)], [(VT, tc)])
                pending = []

                def make_epilogue(ti, t0, n, isctx):
                    wsel = 2 if isctx else w

                    def part1():
                        tt("dve", ON[0].t[:, :n], ON[0].t[:, :n], R1[0].t[:, :n], ALU.mult, [(ON[0], None), (R1[0], None)], [(ON[0], None)])
                        stt(ON[1].t[:, :n], ON[1].t[:, :n], NEGLAM, R1[1].t[:, :n], ALU.mult, ALU.mult, [(ON[1], None), (R1[1], None), (LAMS, None)], [(ON[1], None)])
                        tt("dve", OF.t[:, :n], ON[0].t[:, :n], ON[1].t[:, :n], ALU.add, [(ON[0], None), (ON[1], None)], [(OF, None)])
                        act(OSQ.t[:, :n], OF.t[:, :n], AF.Square, [(OF, None)], [(OSQ, None)])
                        rstd_to_psum([OSQ.t[:, :n]], M128, PS[1], R1[0], n, SUBLN_EPS, [(OSQ, None)])
                        stt(OHT.t[:, :n], OF.t[:, :n], SW, R1[0].t[:, :n], ALU.mult, ALU.mult, [(OF, None), (LAMS, None), (R1[0], None)], [(OHT, None)])

                    def part2():
                        for c in range(KC):
                            py = PS[1 + c % 2]
                            mm(py, py.t[:, :n], woh.t[:, c * 128:(c + 1) * 128], OHT.t[:, :n], True, True, [(woh, None), (OHT, None)])
                            stt(H.t[:, c, t0:t0 + n], py.t[:, :n], GCOEF.t[:, l, 1, wsel, c:c + 1], H.t[:, c, t0:t0 + n], ALU.mult, ALU.add,
                                [(py, None), (GCOEF, None), (H, ti)], [(H, ti)])
                    return [part1, part2]

                for (ti, (t0, n, isctx)) in qtiles:
                    kchunks = list(range(cfg.cchunks)) if isctx else list(range(NCH))
                    for m in range(2):
                        po, psm = PS[3], PS[4]
                        nk = len(kchunks)

                        SB_ = [PS[5], PS[6], PS[7], PS[0]]

                        def s_mm(ki):
                            kc_ = kchunks[ki]
                            pS = SB_[ki % 4]
                            mm(pS, pS.t[:, :n], KT.t[:, kc_ * 128:(kc_ + 1) * 128], QTM[m].t[:, t0:t0 + n],
                               True, True, [(KT, None), (QTM[m], None)])

                        for k0 in range(min(3, nk)):
                            s_mm(k0)
                        for ki, kc_ in enumerate(kchunks):
                            pS = SB_[ki % 4]
                            pt = PT[ki % 3]
                            act(pt.t[:, :n], pS.t[:, :n], AF.Exp, [(pS, None)], [(pt, None)], scale=HD ** -0.5)
                            if ki + 3 < nk:
                                s_mm(ki + 3)
                            mm(po, po.t[:, :n], VT.t[:, kc_, :], pt.t[:, :n], ki == 0, ki == nk - 1, [(VT, None), (pt, None)])
                            mm(psm, psm.t[:, :n], ONESB.t[:], pt.t[:, :n], ki == 0, ki == nk - 1, [(ONESB, None), (pt, None)])
                            if m == 0 and pending and (ki == min(3, nk - 1) or ki == min(9, nk - 1)):
                                pending.pop(0)()
                        cp("dve", ON[m].t[:, :n], po.t[:, :n], [(po, None)], [(ON[m], None)])
                        act(LNT.t[:, :n], psm.t[:, :n], AF.Ln, [(psm, None)], [(LNT, None)])
                        act(R1[m].t[:, :n], LNT.t[:, :n], AF.Exp, [(LNT, None)], [(R1[m], None)], scale=-1.0)
                    while pending:
                        pending.pop(0)()
                    pending.extend(make_epilogue(ti, t0, n, isctx))
                while pending:
                    pending.pop(0)()

        def mlstm(l, seq, need_ctx):
            phase()
            mi = l // 2
            w = seq
            dscale = DH_M ** -0.5
            CONV = sb("CONV", [128, NM * 6 * 16])
            SKIPT = sb("SKIPT", [128, NM * 16])
            MNW = sb("MNW", [128, NM * 16])
            BD = sb("BD", [128, 3, 16, 128], BF16)
            WG = sb("WG", [128, 48, 16], BF16)
            WGC = sb("WGC", [128, 2, 16, 16], BF16)
            BGROW = sb("BGROW", [1, 16])
            GT = sb("GT", [128, NCH, 16])
            NL = sb("NL", [128, 2, NCH, 4])
            IG = sb("IG", [128, 2, NCH, 4])
            GB = sb("GB", [128, 2, NCH, 4])
            GIMB = sb("GIMB", [128, 2, NCH, 4])
            GW = sb("GW", [128, 2, NCH, 4])
            GEB = sb("GEB", [128, 2, NCH, 4])
            GDEC = sb("GDEC", [128, 2, NCH, 4])
            common_off = ar["off"]
            BDT = sb("BDT", [128, 3, 16, 128], BF16)
            dma("sp", CONV.t[:], m_convT, [], [(CONV, None)])
            dma("sp", SKIPT.t[:], m_skipT, [], [(SKIPT, None)])
            dma("sp", MNW.t[:], m_nwT, [], [(MNW, None)])
            cv = lambda tap, fc: CONV.t[:, (mi * 6 + tap) * 16 + fc:(mi * 6 + tap) * 16 + fc + 1]
            dma("pool", BD.t[:], m_bd[mi].rearrange("a c k o -> k a c o"), [], [(BD, None)])
            dma("pool", BDT.t[:], m_bdT[mi].rearrange("a c o k -> o a c k"), [], [(BDT, None)])
            dma("pool", WG.t[:], m_wg[mi].rearrange("(c p) g -> p c g", p=128), [], [(WG, None)])
            dma("sp", BGROW.t[:], m_bg[mi:mi + 1, :], [], [(BGROW, None)])
            for fc in range(16):
                pg = PS[0]
                mm(pg, pg.t[:, fc * 32:fc * 32 + 16], BDT.t[:, 0, fc, :], WG.t[:, fc, :], True, False, [(BDT, None), (WG, None)])
                mm(pg, pg.t[:, fc * 32:fc * 32 + 16], BDT.t[:, 1, fc, :], WG.t[:, 16 + fc, :], False, True, [(BDT, None), (WG, None)])
                mm(pg, pg.t[:, fc * 32 + 16:fc * 32 + 32], BDT.t[:, 2, fc, :], WG.t[:, 32 + fc, :], True, True, [(BDT, None), (WG, None)])
            pgv = PS[0].t[:, :].rearrange("p (c two g) -> p two c g", two=2, g=16)
            for two in range(2):
                cp("dve", WGC.t[:, two, :, :], pgv[:, two, :, :], [(PS[0], None)], [(WGC, None)])
            phase()
            ar["off"] = common_off
            WUP = [sb("WUP%d" % i, [128, KC, 128], BF16) for i in range(2)]
            XINB = [sb("XINB%d" % i, [128, S], BF16) for i in range(2)]
            XCB = [sb("XCB%d" % i, [128, S], BF16) for i in range(2)]
            DG = [sb("DG%d" % i, [128, 5, 128], BF16) for i in range(2)]
            VB = sb("VB", [128, NCH, 512], BF16)
            segs = [(0, NCTX), (NCTX, S)]
            for fc in range(16):
                head, fi = fc // 4, fc % 4
                wu, xinb, xcb, dg = WUP[fc % 2], XINB[fc % 2], XCB[fc % 2], DG[fc % 2]
                dma("pool", wu.t[:], m_wup[mi].rearrange("(kc p) n -> p kc n", p=128)[:, :, fc * 128:(fc + 1) * 128], [], [(wu, None)])
                for tap in range(5):
                    ts("pool" if tap % 2 else "dve", dg.t[:, tap, :], IDENT, cv(tap, fc), None, ALU.mult, None, [(CST, None), (CONV, None)], [(dg, None)])
                for ti, (t0, n, isctx) in enumerate(cfg.tiles):
                    if fc == 0:
                        if ti == 0:
                            make_u_tile(l, 1, seq, 0)
                        if ti + 1 < len(cfg.tiles):
                            make_u_tile(l, 1, seq, ti + 1)
                    px = PS[ti % 2]
                    for kc in range(KC):
                        mm(px, px.t[:, :n], wu.t[:, kc, :], U.t[:, kc, t0:t0 + n], kc == 0, kc == KC - 1, [(wu, None), (U, ti)])
                    cp("act", xinb.t[:, t0:t0 + n], px.t[:, :n], [(px, None)], [(xinb, ti)])
                for ti, (t0, n, isctx) in enumerate(cfg.tiles):
                    s0, s1 = segs[0] if isctx else segs[1]
                    pc = PS[2 + ti % 2]
                    taps = [2, 0, 1, 3, 4]
                    for k_, tap in enumerate(taps):
                        d = tap - 2
                        a0, a1 = max(t0, s0 - d), min(t0 + n, s1 - d)
                        mm(pc, pc.t[:, a0 - t0:a1 - t0], dg.t[:, tap, :], xinb.t[:, a0 + d:a1 + d], k_ == 0, k_ == 4, [(dg, None), (xinb, None)])
                    act(xcb.t[:, t0:t0 + n], pc.t[:, :n], AF.Silu, [(pc, None), (CONV, None)], [(xcb, ti)], bias=cv(5, fc))
                dma("sp", xcs[fc], xcb.t[:], [(xcb, None)], [(XCS, fc)])
                for tc in range(NCH):
                    pv = PS[4 + tc % 2]
                    mm(pv, pv.t[:, :128], xinb.t[:, tc * 128:(tc + 1) * 128], BD.t[:, 2, fc, :], True, True, [(xinb, None), (BD, None)])
                    cp("dve", VB.t[:, tc, fi * 128:(fi + 1) * 128], pv.t[:, :128], [(pv, None)], [(VB, None)])
                    pgt = PS[6]
                    first = True
                    if fc == 0:
                        mm(pgt, pgt.t[:, tc * 16:(tc + 1) * 16], ONEROW.t[:], BGROW.t[:], True, False, [(ONEROW, None), (BGROW, None)])
                        first = False
                    mm(pgt, pgt.t[:, tc * 16:(tc + 1) * 16], xcb.t[:, tc * 128:(tc + 1) * 128], WGC.t[:, 0, fc, :], first, False, [(xcb, None), (WGC, None)])
                    mm(pgt, pgt.t[:, tc * 16:(tc + 1) * 16], xinb.t[:, tc * 128:(tc + 1) * 128], WGC.t[:, 1, fc, :], False, True, [(xinb, None), (WGC, None)])
                gtv = GT.t[:].rearrange("p c g -> p (c g)")
                if fc == 0:
                    cp("dve", gtv, PS[6].t[:, :NCH * 16], [(PS[6], None)], [(GT, None)])
                else:
                    tt("dve", gtv, gtv, PS[6].t[:, :NCH * 16], ALU.add, [(PS[6], None), (GT, None)], [(GT, None)])
                if fi == 3:
                    dma("sp", vs[head], VB.t[:], [(VB, None)], [(VS, head)])
            for d_ in range(2):
                cp("dve", IG.t[:, d_, :, :], GT.t[:, :, d_ * 8:d_ * 8 + 4], [(GT, None)], [(IG, None)])
                act(NL.t[:, d_, :, :], GT.t[:, :, d_ * 8 + 4:d_ * 8 + 8], AF.Exp, [(GT, None)], [(NL, None)], scale=-1.0)
            nlall = NL.t[:].rearrange("p a c h -> p (a c h)")
            act(nlall, nlall, AF.Ln, [(NL, None)], [(NL, None)], bias=1.0)
            for d_ in range(2):
                tri = TRIF if d_ == 0 else TRIB
                fl = lambda B_: B_.t[:, d_, :, :].rearrange("p c h -> p (c h)")
                pb_, pt_ = PS[0], PS[1]
                mm(pb_, pb_.t[:, :NCH * 4], tri, fl(NL), True, True, [(CST, None), (NL, None)])
                mm(pt_, pt_.t[:, :NCH * 4], ONES32.t[:], fl(NL), True, True, [(ONES32, None), (NL, None)])
                cp("dve", fl(GB), pb_.t[:, :NCH * 4], [(pb_, None)], [(GB, None)])
                tt("dve", fl(GIMB), fl(IG), fl(GB), ALU.add, [(IG, None), (GB, None)], [(GIMB, None)])
                tt("dve", fl(GW), fl(GIMB), pt_.t[:, :NCH * 4], ALU.subtract, [(GIMB, None), (pt_, None)], [(GW, None)])
                act(fl(GW), fl(GW), AF.Exp, [(GW, None)], [(GW, None)])
                ts("dve", fl(GW), fl(GW), dscale, None, ALU.mult, None, [(GW, None)], [(GW, None)])
                act(fl(GIMB), fl(GIMB), AF.Exp, [(GIMB, None)], [(GIMB, None)])
                act(fl(GEB), fl(GB), AF.Exp, [(GB, None)], [(GEB, None)], scale=-1.0)
                act(fl(GDEC), pt_.t[:, :NCH * 4], AF.Exp, [(pt_, None)], [(GDEC, None)], scale=-1.0)
            for head in range(NH_M):
                phase()
                ar["off"] = common_off
                ST = []
                for d_ in range(2):
                    st = dict(
                        XCC=[sb("XCC%d" % i, [128, 4, 128], BF16) for i in range(2)],
                        VC=[sb("VC%d" % i, [128, 512], BF16) for i in range(2)],
                        HB=[sb("HB%d" % i, [128, 512]) for i in range(2)],
                        CTS=sb("CTS", [128, 4, 512]), CTB=sb("CTB", [128, 4, 512], BF16),
                        NV=sb("NV", [128, 4]), NVB=sb("NVB", [128, 4], BF16),
                        QC=sb("QC", [128, 4, 128], BF16), KCB=sb("KCB", [128, 4, 128], BF16), KW=sb("KW", [128, 512], BF16),
                        SD=sb("SD", [128, 128], BF16), DEN=sb("DEN", [128, 8]),
                        X=[PS[4 * d_], PS[4 * d_ + 1]], Y=PS[4 * d_ + 2], N=PS[4 * d_ + 3])
                    ST.append(st)

                def scan(d_):
                    st = ST[d_]
                    order = list(range(NCH)) if d_ == 0 else (list(range(cfg.cchunks - 1, -1, -1)) + list(range(NCH - 1, cfg.cchunks - 1, -1)))
                    mask = TRIF if d_ == 0 else TRIB
                    CTS, CTB, NV, NVB, QC, KCB, KW, SD, DEN = (st[k] for k in ("CTS", "CTB", "NV", "NVB", "QC", "KCB", "KW", "SD", "DEN"))
                    X0, X1, Y, N_ = st["X"][0], st["X"][1], st["Y"], st["N"]
                    memset("pool", CTS, CTS.t[:], 0.0)
                    memset("pool", CTB, CTB.t[:], 0.0)
                    memset("pool", NV, NV.t[:], 0.0)
                    memset("pool", NVB, NVB.t[:], 0.0)
                    yield
                    for oi, tc in enumerate(order):
                        col = lambda B_: B_.t[:, d_, tc, head:head + 1]
                        xcc, vc, hb = st["XCC"][oi % 2], st["VC"][oi % 2], st["HB"][oi % 2]
                        dma("sp", xcc.t[:], xcs[head * 4:(head + 1) * 4, :, tc * 128:(tc + 1) * 128].rearrange("c p s -> p c s"),
                            [(XCS, None)], [(xcc, None)])
                        dma("sp", vc.t[:], vs[head, :, tc, :], [(VS, head)], [(vc, None)])
                        for i in range(4):
                            mm(X0, X0.t[:, i * 128:(i + 1) * 128], BD.t[:, 0, head * 4 + i, :], xcc.t[:, i, :], True, True, [(BD, None), (xcc, None)])
                        cp("act", QC.t[:].rearrange("p a b -> p (a b)"), X0.t[:, :], [(X0, None)], [(QC, None)])
                        for i in range(4):
                            mm(X1, X1.t[:, i * 128:(i + 1) * 128], BD.t[:, 1, head * 4 + i, :], xcc.t[:, i, :], True, True, [(BD, None), (xcc, None)])
                        ts("dve", KCB.t[:].rearrange("p a b -> p (a b)"), X1.t[:, :], dscale, None, ALU.mult, None, [(X1, None)], [(KCB, None)])
                        yield
                        for i in range(4):
                            mm(X0, X0.t[:, i * 128:(i + 1) * 128], xcc.t[:, i, :], BD.t[:, 1, head * 4 + i, :], True, True, [(BD, None), (xcc, None)])
                        act(KW.t[:], X0.t[:, :], AF.Identity, [(X0, None), (GW, None)], [(KW, None)], scale=col(GW))
                        for i in range(4):
                            mm(Y, Y.t[:, :128], KCB.t[:, i, :], QC.t[:, i, :], i == 0, i == 3, [(KCB, None), (QC, None)])
                        stt(SD.t[:], Y.t[:, :128], col(GIMB), mask, ALU.mult, ALU.mult, [(Y, None), (GIMB, None), (CST, None)], [(SD, None)])
                        yield
                        mm(N_, N_.t[:, :], SD.t[:], vc.t[:], True, False, [(SD, None), (vc, None)])
                        for i in range(4):
                            mm(N_, N_.t[:, :], QC.t[:, i, :], CTB.t[:, i, :], False, i == 3, [(QC, None), (CTB, None)])
                        mm(Y, Y.t[:, 256:257], SD.t[:], ONESB.t[:, 0:1], True, False, [(SD, None), (ONESB, None)])
                        for i in range(4):
                            mm(Y, Y.t[:, 256:257], QC.t[:, i, :], NVB.t[:, i:i + 1], False, i == 3, [(QC, None), (NVB, None)])
                        act(DEN.t[:, 0:1], Y.t[:, 256:257], AF.Abs, [(Y, None), (GEB, None)], [(DEN, None)], scale=col(GEB))
                        ts("dve", DEN.t[:, 1:2], DEN.t[:, 0:1], 1.0, None, ALU.max, None, [(DEN, None)], [(DEN, None)])
                        sch.op("dve", lambda e: e.reciprocal(out=DEN.t[:, 2:3], in_=DEN.t[:, 1:2]), reads=[(DEN, None)], writes=[(DEN, None)])
                        tt("dve", DEN.t[:, 3:4], DEN.t[:, 2:3], col(GEB), ALU.mult, [(DEN, None), (GEB, None)], [(DEN, None)])
                        act(hb.t[:], N_.t[:, :], AF.Identity, [(N_, None), (DEN, None)], [(hb, None)], scale=DEN.t[:, 3:4])
                        dma("sp", hfs[d_, tc], hb.t[:], [(hb, None)], [(HFS, (d_, tc))])
                        yield
                        for i in range(4):
                            pu = st["X"][(i + 1) % 2]
                            mm(pu, pu.t[:, :], KW.t[:, i * 128:(i + 1) * 128], vc.t[:], True, True, [(KW, None), (vc, None)])
                            stt(CTS.t[:, i, :], CTS.t[:, i, :], col(GDEC), pu.t[:, :], ALU.mult, ALU.add, [(CTS, i), (GDEC, None), (pu, None)], [(CTS, i)])
                            cp("act", CTB.t[:, i, :], CTS.t[:, i, :], [(CTS, i)], [(CTB, i)])
                            if i % 2 == 1:
                                yield
                        for i in range(4):
                            mm(Y, Y.t[:, 264 + 2 * i:265 + 2 * i], KW.t[:, i * 128:(i + 1) * 128], ONESB.t[:, 0:1], True, True, [(KW, None), (ONESB, None)])
                        pnv = Y.t[:, 264:272].rearrange("p (a b) -> p a b", b=2)[:, :, 0]
                        stt(NV.t[:], NV.t[:], col(GDEC), pnv, ALU.mult, ALU.add, [(NV, None), (GDEC, None), (Y, None)], [(NV, None)])
                        cp("dve", NVB.t[:], NV.t[:], [(NV, None)], [(NVB, None)])
                        yield

                gens = [scan(0), scan(1)]
                alive = [True, True]
                while any(alive):
                    for gi, g in enumerate(gens):
                        if alive[gi]:
                            try:
                                next(g)
                            except StopIteration:
                                alive[gi] = False
                phase()
                ar["off"] = common_off
                WUZ = sb("WUZ", [128, KC, 512], BF16)
                WD = sb("WD", [128, 4, D], BF16)
                XCO = [sb("XCO%d" % i, [128, 4, 256], BF16) for i in range(2)]
                HF = [sb("HF%d" % i, [128, 2, 512]) for i in range(2)]
                HBW = [sb("HBW%d" % i, [128, 2, 512]) for i in range(2)]
                BNS_ = [sb("BNS%d" % i, [128, 2, 8]) for i in range(2)]
                RS_ = [sb("RS%d" % i, [128, 4]) for i in range(2)]
                TN_ = [sb("TN%d" % i, [128, 4, 256]) for i in range(2)]
                SZ = sb("SZ", [128, 4, 256])
                MT = sb("MT", [128, 4, 256], BF16)
                dma("pool", WUZ.t[:], m_wup[mi].rearrange("(kc p) n -> p kc n", p=128)[:, :, INNER + head * 512:INNER + (head + 1) * 512],
                    [], [(WUZ, None)])
                dma("pool", WD.t[:], m_wdown[mi].rearrange("(c p) d -> p c d", p=128)[:, head * 4:(head + 1) * 4, :], [], [(WD, None)])
                ogroups = []
                for (c0, c1) in ((0, cfg.cchunks), (cfg.cchunks, NCH)):
                    if c0 == 0 and not need_ctx:
                        continue
                    tc = c0
                    while tc < c1:
                        ogroups.append(list(range(tc, min(tc + 2, c1))))
                        tc += 2
                for gi_, grp in enumerate(ogroups):
                    par = gi_ % 2
                    ng = len(grp)
                    T = 128 * ng
                    g0 = grp[0]
                    tok = slice(g0 * 128, g0 * 128 + T)
                    xco, hf, hbw, BNS, RS, TN = XCO[par], HF[par], HBW[par], BNS_[par], RS_[par], TN_[par]
                    isctx = g0 < cfg.cchunks
                    ti = [k for k, (a, n_, _) in enumerate(cfg.tiles) if a <= g0 * 128 < a + n_][0]
                    wsel = 2 if isctx else w
                    dma("sp", xco.t[:, :, :T], xcs[head * 4:(head + 1) * 4, :, g0 * 128:g0 * 128 + T].rearrange("c p s -> p c s"), [(XCS, None)], [(xco, None)])
                    dma("sp", hf.t[:, :ng, :], hfs[0, g0:g0 + ng].rearrange("c p f -> p c f"), [(HFS, None)], [(hf, None)])
                    dma("sp", hbw.t[:, :ng, :], hfs[1, g0:g0 + ng].rearrange("c p f -> p c f"), [(HFS, None)], [(hbw, None)])
                    tt("pool", hf.t[:, :ng, :], hf.t[:, :ng, :], hbw.t[:, :ng, :], ALU.add, [(hf, None), (hbw, None)], [(hf, None)])
                    tt("pool", TN.t[:, :, :T], xco.t[:, :, :T], SKIPT.t[:, mi * 16 + head * 4:mi * 16 + head * 4 + 4].unsqueeze(2).to_broadcast([128, 4, T]), ALU.mult,
                       [(xco, None), (SKIPT, None)], [(TN, None)])
                    pzb = lambda i: PS[i // 2].t[:, (i % 2) * 256:(i % 2) * 256 + T]
                    phb = lambda i: PS[2 + i // 2].t[:, (i % 2) * 256:(i % 2) * 256 + T]
                    for i in range(4):
                        for kc in range(KC):
                            mm(PS[i // 2], pzb(i), WUZ.t[:, kc, i * 128:(i + 1) * 128], U.t[:, kc, tok], kc == 0, kc == KC - 1, [(WUZ, None), (U, ti)])
                    for j in range(ng):
                        sch.op("dve", lambda e, hf=hf, BNS=BNS, j=j: e.bn_stats(out=BNS.t[:, j, 0:6], in_=hf.t[:, j, :]), reads=[(hf, None)], writes=[(BNS, None)])
                        sch.op("dve", lambda e, BNS=BNS, j=j: e.bn_aggr(out=BNS.t[:, j, 6:8], in_=BNS.t[:, j, 0:6]), reads=[(BNS, None)], writes=[(BNS, None)])
                    act(RS.t[:, 0:ng], BNS.t[:, 0:ng, 7], AF.Ln, [(BNS, None)], [(RS, None)], bias=HEAD_LN_EPS)
                    act(RS.t[:, 0:ng], RS.t[:, 0:ng], AF.Exp, [(RS, None)], [(RS, None)], scale=-0.5)
                    for j in range(ng):
                        ts("dve", hf.t[:, j, :], hf.t[:, j, :], BNS.t[:, j, 6:7], RS.t[:, j:j + 1], ALU.subtract, ALU.mult, [(hf, None), (BNS, None), (RS, None)], [(hf, None)])
                    for b in range(2):
                        pzv = PS[b].t[:, 0:512].rearrange("p (a t) -> p a t", a=2)[:, :, :T]
                        szv = SZ.t[:, 2 * b:2 * b + 2, :T]
                        act(szv, pzv, AF.Exp, [(PS[b], None)], [(SZ, b)], scale=-1.0)
                        act(szv, szv, AF.Ln, [(SZ, b)], [(SZ, b)], bias=1.0)
                        act(szv, szv, AF.Exp, [(SZ, b)], [(SZ, b)], scale=-1.0)
                        stt(szv, pzv, 1.0, szv, ALU.mult, ALU.mult, [(PS[b], None), (SZ, b)], [(SZ, b)])
                    for j in range(ng):
                        for i in range(4):
                            tr(PS[2 + i // 2], PS[2 + i // 2].t[:, (i % 2) * 256 + j * 128:(i % 2) * 256 + (j + 1) * 128],
                               hf.t[:, j, i * 128:(i + 1) * 128], IDENT, [(hf, None), (CST, None)])
                    for i in range(4):
                        fcg = mi * 16 + head * 4 + i
                        stt(TN.t[:, i, :T], phb(i), MNW.t[:, fcg:fcg + 1], TN.t[:, i, :T], ALU.mult, ALU.add,
                            [(PS[2 + i // 2], None), (MNW, None), (TN, None)], [(TN, None)])
                    tt("dve", MT.t[:, :, :T], TN.t[:, :, :T], SZ.t[:, :, :T], ALU.mult, [(TN, None), (SZ, None)], [(MT, None)])
                    for c in range(KC):
                        py = PS[4 + c % 4]
                        for i in range(4):
                            mm(py, py.t[:, :T], WD.t[:, i, c * 128:(c + 1) * 128], MT.t[:, i, :T], i == 0, i == 3, [(WD, None), (MT, None)])
                        stt(H.t[:, c, tok], py.t[:, :T], GCOEF.t[:, l, 1, wsel, c:c + 1], H.t[:, c, tok], ALU.mult, ALU.add,
                            [(py, None), (GCOEF, None), (H, ti)], [(H, ti)])

        for seq in range(cfg.nseq):
            phase()
            for ti, (t0, n, isctx) in enumerate(cfg.tiles):
                dma("sp", H.t[:, :, t0:t0 + n], xT[seq].rearrange("(kc p) s -> p kc s", p=128)[:, :, t0:t0 + n], [], [(H, ti)])
            for l in cfg.layers:
                last = (l == cfg.depth - 1)
                ffn(l, 0, 0, seq, True)
                if l % 2 == 0:
                    attention(l, seq, not last)
                else:
                    mlstm(l, seq, not last)
                ffn(l, 1, 2, seq, not last)
            phase()
            OUTB = [sb("OUTB%d" % i, [128, 256]) for i in range(2)]
            cnt = [0]

            def fin(ti, t0, n, isctx, c, tb):
                if cfg.final and isctx:
                    return
                ob = OUTB[cnt[0] % 2]
                cnt[0] += 1
                if cfg.final:
                    ts("dve", ob.t[:, :n], tb.t[:, :n], FNW.t[:, c:c + 1], None, ALU.mult, None, [(tb, None), (FNW, None)], [(ob, None)])
                    dma("sp", outT[seq, c * 128:(c + 1) * 128, t0 - NCTX:t0 - NCTX + n], ob.t[:, :n], [(ob, None)], [])
            if cfg.final:
                make_u(0, 0, seq, out_fn=fin)
            else:
                for ti, (t0, n, isctx) in enumerate(cfg.tiles):
                    dma("sp", outT[seq].rearrange("(kc p) s -> p kc s", p=128)[:, :, t0:t0 + n], H.t[:, :, t0:t0 + n], [(H, ti)], [])
        sch.emit()
    return nc


def _rope_tables(nlat):
    rows = nlat // GRID_W
    row_pos = np.repeat(np.arange(rows, dtype=np.float32), GRID_W)
    col_pos = np.tile(np.arange(GRID_W, dtype=np.float32), rows)
    half = HD // 2
    inv_freq = (1.0 / (ROPE_THETA ** (np.arange(0, half, 2, dtype=np.float32) / half))).astype(np.float32)
    C = np.zeros((128, nlat), np.float32)
    Sg = np.zeros((128, nlat), np.float32)
    for p in range(128):
        d = p % 64
        axis, r, fq = d // 32, (d % 32) // 16, d % 16
        ang = (row_pos if axis == 0 else col_pos) * inv_freq[fq]
        C[p] = np.cos(ang)
        Sg[p] = np.sin(ang) * (-1.0 if r == 0 else 1.0)
    return C, Sg


def _consts():
    s = np.arange(128)[:, None]
    t = np.arange(128)[None, :]
    ident = np.eye(128, dtype=np.float32)
    triF = (s <= t).astype(np.float32)
    triB = (s >= t).astype(np.float32)
    mF = np.where(s <= t, 0.0, -NEG).astype(np.float32)
    mB = np.where(s >= t, 0.0, -NEG).astype(np.float32)
    return np.stack([ident, triF, triB, mF, mB]).astype(np.float32)


def _prep_shared(inp, cfg):
    L = cfg.depth
    f = lambda a: np.ascontiguousarray(np.asarray(a, dtype=np.float32))
    sh = {}
    sh["w_mod"] = f(inp["w_mod"])
    sh["bmodT"] = f(np.asarray(inp["b_mod"]).reshape(L, 72, 128).transpose(0, 2, 1))
    sh["normwT"] = f(np.asarray(inp["norm_w"]).reshape(L, 3, 8, 128).transpose(3, 0, 1, 2).reshape(128, L * 24))
    sh["fnormT"] = f(np.asarray(inp["final_norm_w"]).reshape(8, 128).T)
    sh["ffn_w_in"] = f(inp["ffn_w_in"])
    sh["ffn_w_out"] = f(inp["ffn_w_out"])
    NA, NM = max(cfg.n_attn, 1), max(cfg.n_ml, 1)
    wqkv = np.asarray(inp["attn_w_qkv"], dtype=np.float32)
    na = wqkv.shape[0]
    p = np.arange(128)
    d = p % 64
    partner = np.where((d % 32) < 16, p + 16, p - 16)
    ext = np.zeros((NA, D, 8, 5, 128), np.float32)
    for a in range(min(na, NA)):
        q = wqkv[a][:, 0:D].reshape(D, 8, 128)
        k = wqkv[a][:, D:2 * D].reshape(D, 8, 128)
        v = wqkv[a][:, 2 * D:3 * D].reshape(D, 8, 128)
        ext[a, :, :, 0] = q
        ext[a, :, :, 1] = q[:, :, partner]
        ext[a, :, :, 2] = k
        ext[a, :, :, 3] = k[:, :, partner]
        ext[a, :, :, 4] = v
    sh["a_wext"] = ext
    pad = lambda a, n: f(np.concatenate([np.asarray(a, np.float32)] + [np.asarray(a, np.float32)[:1]] * (n - np.asarray(a).shape[0]), 0)) if np.asarray(a).shape[0] < n else f(np.asarray(a)[:n])
    sh["a_wo"] = pad(inp["attn_w_o"], NA)
    sh["a_lam"] = pad(np.asarray(inp["attn_lambda"]).reshape(-1, 256), NA)
    sh["a_subln"] = f(pad(inp["attn_subln_w"], NA).T)
    C, Sg = _rope_tables(cfg.nlat)
    sh["ropeC"], sh["ropeS"] = C, Sg
    sh["m_wup"] = pad(inp["mlstm_w_up"], NM)
    cw = pad(inp["mlstm_conv_w"], NM)
    cb = pad(inp["mlstm_conv_b"], NM)
    cc = np.concatenate([cw, cb[:, None, :]], 1)
    sh["m_convT"] = f(cc.reshape(NM, 6, 16, 128).transpose(3, 0, 1, 2).reshape(128, NM * 96))
    wq = pad(inp["mlstm_w_qkv"], NM)
    bd = np.zeros((NM, 3, 16, 128, 128), np.float32)
    for c in range(16):
        for g in range(32):
            bd[:, :, c, g * 4:(g + 1) * 4, g * 4:(g + 1) * 4] = wq[:, :, c * 32 + g]
    sh["m_bd"] = bd
    sh["m_bdT"] = f(bd.transpose(0, 1, 2, 4, 3))
    sh["m_wg"] = pad(inp["mlstm_w_gates"], NM)
    sh["m_bg"] = pad(inp["mlstm_b_gates"], NM)
    sh["m_skipT"] = f(pad(inp["mlstm_skip"], NM).reshape(NM, 16, 128).transpose(2, 0, 1).reshape(128, NM * 16))
    sh["m_nwT"] = f(pad(inp["mlstm_norm_w"], NM).reshape(NM, 16, 128).transpose(2, 0, 1).reshape(128, NM * 16))
    sh["m_wdown"] = pad(inp["mlstm_w_down"], NM)
    sh["cst"] = _consts()
    return sh


def run_cfg(inp, cfg, n_cores=8):
    x = np.asarray(inp["x"], np.float32)
    ctx = np.asarray(inp["ctx"], np.float32)
    c = np.asarray(inp["c"], np.float32)
    c_ctx = np.asarray(inp["c_ctx"], np.float32)
    B = x.shape[0]
    assert B == n_cores * cfg.nseq
    sh = _prep_shared(inp, cfg)
    nc = build_program(cfg)
    in_maps = []
    for core in range(n_cores):
        bs = [core * cfg.nseq + i for i in range(cfg.nseq)]
        xt = np.stack([np.concatenate([ctx[b], x[b]], 0).T for b in bs]).astype(np.float32)
        cols = [c[bs[i]] if i < cfg.nseq else c[bs[0]] for i in range(2)] + [c_ctx]
        m = dict(sh)
        m["xT"] = np.ascontiguousarray(xt)
        m["cT"] = np.ascontiguousarray(np.stack(cols, 1).astype(np.float32))
        in_maps.append(m)
    res = run_bass_kernel_spmd(nc, in_maps, core_ids=list(range(n_cores)))
    outs = []
    for core in range(n_cores):
        o = np.asarray(res.results[core]["outT"])
        for i in range(cfg.nseq):
            outs.append(o[i].T)
    return np.ascontiguousarray(np.stack(outs).astype(np.float32))


def kernel(**inputs):
    cfg = Cfg()
    return run_cfg(inputs, cfg)
```

```python
import math
from contextlib import ExitStack
import numpy as np
import concourse.bass as bass
import concourse.mybir as mybir
from concourse.bass_utils import run_bass_kernel_spmd

F32 = mybir.dt.float32
BF16 = mybir.dt.bfloat16
AF = mybir.ActivationFunctionType
ALU = mybir.AluOpType

D = 1024
KC = 8
DFF = 2816
NFF = 22
NMOD = 9
HD = 64
INNER = 2048
NH_M = 4
DH_M = 512
RMS_EPS = 1e-6
SUBLN_EPS = 1e-5
HEAD_LN_EPS = 1e-5
GRID_W = 64
ROPE_THETA = 10000.0
NEG = -30000.0


class Buf:
    def __init__(self, name, t):
        self.name = name
        self.t = t

    def __getitem__(self, idx):
        return self.t[idx]


class Sched:
    ENGS = ["pe", "act", "dve", "pool", "sp"]
    RING = 16

    def __init__(self, nc):
        self.nc = nc
        self.ops = {e: [] for e in self.ENGS}
        self.state = {}

    def _st(self, buf):
        return self.state.setdefault(buf.name, {})

    def _deps(self, buf, key, is_write):
        st = self._st(buf)
        keys = list(st.keys()) if key is None else [k for k in (key, None) if k in st]
        out = set()
        for k2 in keys:
            s = st[k2]
            if s["w"] is not None:
                out.add(s["w"])
            if is_write:
                for e, i in s["r"].items():
                    out.add((e, i))
        return out

    def op(self, eng, fn, reads=(), writes=(), dma=False, barrier=False):
        idx = len(self.ops[eng])
        deps = set()
        for (b, k) in reads:
            deps |= self._deps(b, k, False)
        for (b, k) in writes:
            deps |= self._deps(b, k, True)
        if eng == "pe":
            deps = {d for d in deps if d[0] != "pe"}
        deps.discard((eng, idx))
        self.ops[eng].append(dict(fn=fn, deps=deps, dma=dma, inc=False, barrier=barrier))
        for (b, k) in reads:
            st = self._st(b)
            s = st.setdefault(k, {"w": None, "r": {}})
            s["r"][eng] = idx
        for (b, k) in writes:
            st = self._st(b)
            if k is None:
                st.clear()
            st[k] = {"w": (eng, idx), "r": {}}

    def emit(self):
        nc = self.nc
        ops = self.ops
        for e in self.ENGS:
            for rec in ops[e]:
                for (e2, i2) in rec["deps"]:
                    ops[e2][i2]["inc"] = True
        with ExitStack() as es:
            cnt = {e: es.enter_context(nc.semaphore("c_" + e)) for e in ["pe", "act", "dve", "pool"]}
            ring = {e: [es.enter_context(nc.semaphore("r_%s%d" % (e, i))) for i in range(self.RING)]
                    for e in ["sp", "pool"]}
            for e in self.ENGS:
                c = 0
                uses = [0] * self.RING
                nd = 0
                for rec in ops[e]:
                    if rec["dma"]:
                        slot = nd % self.RING
                        nd += 1
                        uses[slot] += 1
                        rec["slot"] = slot
                        rec["tok"] = (("r", e, slot), 16 * uses[slot])
                        rec["pre"] = (("r", e, slot), 16 * (uses[slot] - 1))
                    else:
                        if rec["inc"]:
                            c += 1
                        rec["tok"] = (("c", e), c)
            semof = lambda k: cnt[k[1]] if k[0] == "c" else ring[k[1]][k[2]]
            final_tok = {}
            for e in ["sp", "pool"]:
                for rec in ops[e]:
                    if rec["dma"]:
                        final_tok[rec["tok"][0]] = rec["tok"][1]
            block = es.enter_context(nc.Block())

            def run(e, eng):
                waited = {}
                issued = {}
                for rec in ops[e]:
                    need = {}
                    if rec["barrier"]:
                        need.update(issued)
                    for (e2, i2) in rec["deps"]:
                        k, v = ops[e2][i2]["tok"]
                        need[k] = max(need.get(k, 0), v)
                    if rec["dma"] and rec["pre"][1] > 0:
                        k, v = rec["pre"]
                        need[k] = max(need.get(k, 0), v)
                    for k, v in need.items():
                        if waited.get(k, 0) < v:
                            eng.wait_ge(semof(k), v)
                            waited[k] = v
                    ins = rec["fn"](eng)
                    if rec["dma"]:
                        ins.then_inc(semof(rec["tok"][0]), 16)
                        issued[rec["tok"][0]] = rec["tok"][1]
                    elif rec["inc"]:
                        ins.then_inc(cnt[e], 1)
                if e == "sp":
                    for k, v in final_tok.items():
                        if waited.get(k, 0) < v:
                            eng.wait_ge(semof(k), v)

            @block.tensor
            def _(eng):
                run("pe", eng)

            @block.scalar
            def _(eng):
                run("act", eng)

            @block.vector
            def _(eng):
                run("dve", eng)

            @block.gpsimd
            def _(eng):
                run("pool", eng)

            @block.sync
            def _(eng):
                run("sp", eng)


class Cfg:
    def __init__(self, nctx=256, nlat=2048, layers=(0, 1, 2, 3), nseq=2, final=True, depth=4):
        self.nctx, self.nlat, self.layers, self.nseq, self.final, self.depth = nctx, nlat, tuple(layers), nseq, final, depth
        self.S = nctx + nlat
        self.tiles = []
        t = 0
        while t < nctx:
            n = min(512, nctx - t)
            self.tiles.append((t, n, True))
            t += n
        while t < self.S:
            n = min(512, self.S - t)
            self.tiles.append((t, n, False))
            t += n
        self.nchunk = self.S // 128
        self.cchunks = nctx // 128
        self.n_attn = (depth + 1) // 2
        self.n_ml = depth // 2


def build_program(cfg):
    nc = bass.Bass("TRN2", target_bir_lowering=False)
    S, NCTX, NLAT, NCH = cfg.S, cfg.nctx, cfg.nlat, cfg.nchunk
    L = cfg.depth
    NA, NM = max(cfg.n_attn, 1), max(cfg.n_ml, 1)
    dt = lambda name, shape, dtype=F32, kind="ExternalInput": nc.dram_tensor(name, list(shape), dtype, kind=kind).ap()
    xT = dt("xT", [cfg.nseq, D, S])
    outT = dt("outT", [cfg.nseq, D, NLAT if cfg.final else S], kind="ExternalOutput")
    cT = dt("cT", [D, 3])
    w_mod = dt("w_mod", [L, D, NMOD * D])
    bmodT = dt("bmodT", [L, 128, 72])
    normwT = dt("normwT", [128, L * 3 * 8])
    fnormT = dt("fnormT", [128, 8])
    w_in = dt("ffn_w_in", [L, 2, D, 2 * DFF])
    w_out = dt("ffn_w_out", [L, 2, DFF, D])
    a_wext = dt("a_wext", [NA, D, 8, 5, 128])
    a_wo = dt("a_wo", [NA, D, D])
    a_lam = dt("a_lam", [NA, 256])
    a_subln = dt("a_subln", [128, NA])
    ropeC = dt("ropeC", [128, NLAT])
    ropeS = dt("ropeS", [128, NLAT])
    m_wup = dt("m_wup", [NM, D, 2 * INNER])
    m_convT = dt("m_convT", [128, NM * 6 * 16])
    m_bd = dt("m_bd", [NM, 3, 16, 128, 128])
    m_bdT = dt("m_bdT", [NM, 3, 16, 128, 128])
    m_wg = dt("m_wg", [NM, 3 * INNER, 16])
    m_bg = dt("m_bg", [NM, 16])
    m_skipT = dt("m_skipT", [128, NM * 16])
    m_nwT = dt("m_nwT", [128, NM * 16])
    m_wdown = dt("m_wdown", [NM, INNER, D])
    cst = dt("cst", [5, 128, 128])
    xcs = dt("xcs", [16, 128, S], BF16, kind="Internal")
    vs = dt("vs", [4, 128, NCH, 512], BF16, kind="Internal")
    hfs = dt("hfs", [2, NCH, 128, 512], F32, kind="Internal")

    sch = Sched(nc)
    es = ExitStack()
    HFS, XCS, VS = Buf("d_hfs", None), Buf("d_xcs", None), Buf("d_vs", None)
    with es:
        def sbp(name, shape, dtype=F32):
            return Buf(name, es.enter_context(nc.sbuf_tensor(name, list(shape), dtype)))

        PS = [Buf("ps%d" % i, es.enter_context(nc.psum_tensor("ps%d" % i, [128, 512], F32))) for i in range(8)]
        ARENA_BYTES = 72 * 1024
        ARENA = es.enter_context(nc.sbuf_tensor("ARENA", [128, ARENA_BYTES // 2], BF16))
        ar = {"off": 0, "n": 0}

        def sb(name, shape, dtype=F32):
            esz = 4 if dtype == F32 else 2
            nel = 1
            for s_ in shape[1:]:
                nel *= s_
            nbytes = (nel * esz + 63) // 64 * 64
            off = ar["off"]
            assert off + nbytes <= ARENA_BYTES, (name, off, nbytes)
            ar["off"] = off + nbytes
            v = ARENA[0:shape[0], off // 2:(off + nel * esz) // 2]
            if dtype == F32:
                v = v.bitcast(F32)
            if len(shape) == 3:
                v = v.rearrange("p (a b) -> p a b", a=shape[1])
            elif len(shape) == 4:
                v = v.rearrange("p (a b c) -> p a b c", a=shape[1], b=shape[2])
            ar["n"] += 1
            return Buf("%s#%d" % (name, ar["n"]), v)

        def mm(out_b, out_ap, lhsT, rhs, start, stop, reads, okey=None):
            sch.op("pe", lambda e: e.matmul(out_ap, lhsT, rhs, start=start, stop=stop),
                   reads=reads, writes=[(out_b, okey)])

        def tr(out_b, out_ap, in_ap, ident_ap, reads, okey=None):
            sch.op("pe", lambda e: e.transpose(out_ap, in_ap, ident_ap), reads=reads, writes=[(out_b, okey)])

        def act(out_ap, in_ap, func, reads, writes, bias=None, scale=None):
            kw = {}
            if bias is not None:
                kw["bias"] = bias
            if scale is not None:
                kw["scale"] = scale
            sch.op("act", lambda e: e.activation(out=out_ap, in_=in_ap, func=func, **kw), reads=reads, writes=writes)

        def tt(eng, out_ap, in0, in1, op, reads, writes):
            sch.op(eng, lambda e: e.tensor_tensor(out=out_ap, in0=in0, in1=in1, op=op), reads=reads, writes=writes)

        def ts(eng, out_ap, in0, s1, s2, op0, op1, reads, writes):
            if op1 is None:
                sch.op(eng, lambda e: e.tensor_scalar(out=out_ap, in0=in0, scalar1=s1, scalar2=None, op0=op0),
                       reads=reads, writes=writes)
            else:
                sch.op(eng, lambda e: e.tensor_scalar(out=out_ap, in0=in0, scalar1=s1, scalar2=s2, op0=op0, op1=op1),
                       reads=reads, writes=writes)

        def stt(out_ap, in0, scalar, in1, op0, op1, reads, writes):
            sch.op("dve", lambda e: e.scalar_tensor_tensor(out=out_ap, in0=in0, scalar=scalar, in1=in1, op0=op0, op1=op1),
                   reads=reads, writes=writes)

        def cp(eng, out_ap, in_ap, reads, writes):
            if eng == "act":
                sch.op("act", lambda e: e.copy(out=out_ap, in_=in_ap), reads=reads, writes=writes)
            else:
                sch.op(eng, lambda e: e.tensor_copy(out=out_ap, in_=in_ap), reads=reads, writes=writes)

        def memset(eng, buf, ap, val):
            sch.op(eng, lambda e: e.memset(ap, val), writes=[(buf, None)])

        def dma(q, out_ap, in_ap, reads, writes, **kw):
            sch.op(q, lambda e: e.dma_start(out=out_ap, in_=in_ap, **kw), reads=reads, writes=writes, dma=True)

        H = sbp("H", [128, KC, S])
        U = sbp("U", [128, KC, S], BF16)
        CST = sbp("CST", [128, 5, 128])
        IDENT, TRIF, TRIB, MNEGF, MNEGB = (CST.t[:, i, :] for i in range(5))
        ONES32 = sbp("ONES32", [128, 128])
        ONESB = sbp("ONESB", [128, 128], BF16)
        M1024 = sbp("M1024", [128, 128], BF16)
        M128 = sbp("M128", [128, 128], BF16)
        ONEROW = sbp("ONEROW", [1, 128])
        MHALF = sbp("MHALF", [128, 8])
        MODT = sbp("MODT", [128, L, 72, 3])
        NWT = sbp("NWT", [128, L * 3 * 8])
        FNW = sbp("FNW", [128, 8])
        ACOEF = sbp("ACOEF", [128, L, 3, 3, 8])
        GCOEF = sbp("GCOEF", [128, L, 3, 3, 8])
        SQ = sbp("SQ", [128, KC, 256], BF16)
        LNT = sbp("LNT", [128, 512])
        TMPU = [sbp("TMPU%d" % i, [128, 256]) for i in range(2)]
        DUM = {e: sbp("DUM" + e, [128, 8]) for e in ["act", "dve", "pool", "sp"]}
        BAR = Buf("BAR", None)
        BAR2 = Buf("BAR2", None)

        def phase():
            for stage, bb in ((0, BAR), (1, BAR2)):
                for e in Sched.ENGS:
                    rd = [] if stage == 0 else [(BAR, k) for k in Sched.ENGS if k != e]
                    wr = [(bb, e)]
                    if e == "pe":
                        sch.op("pe", lambda en: en.matmul(PS[7].t[:, 0:2], ONESB.t[:, :], ONESB.t[:, 0:2], start=True, stop=True),
                               reads=rd + [(ONESB, None)], writes=wr + [(PS[7], None)], barrier=(stage == 0))
                    elif e == "sp":
                        sch.op("sp", lambda en: en.dma_start(out=DUM["sp"].t[0:1, 0:8], in_=cst[0, 0:1, 0:8]),
                               reads=rd, writes=wr + [(DUM["sp"], None)], dma=True, barrier=(stage == 0))
                    elif e == "act":
                        sch.op(e, lambda en: en.copy(out=DUM["act"].t[:], in_=ONES32.t[:, 0:8]), reads=rd + [(ONES32, None)],
                               writes=wr + [(DUM[e], None)], barrier=(stage == 0))
                    else:
                        sch.op(e, lambda en, e=e: en.memset(DUM[e].t[:], 0.0), reads=rd, writes=wr + [(DUM[e], None)],
                               barrier=(stage == 0))
            ar["off"] = 0

        dma("sp", CST.t[:], cst.rearrange("a p f -> p a f"), [], [(CST, None)])
        dma("sp", NWT.t[:], normwT, [], [(NWT, None)])
        dma("sp", FNW.t[:], fnormT, [], [(FNW, None)])
        memset("dve", ONES32, ONES32.t[:], 1.0)
        memset("dve", ONESB, ONESB.t[:], 1.0)
        memset("dve", M1024, M1024.t[:], 1.0 / 1024.0)
        memset("dve", M128, M128.t[:], 1.0 / 128.0)
        memset("dve", ONEROW, ONEROW.t[:], 1.0)
        memset("dve", MHALF, MHALF.t[:], -0.5)

        CT3 = sb("CT3", [128, KC, 3])
        SCT = sb("SCT", [128, KC, 3], BF16)
        dma("sp", CT3.t[:], cT.rearrange("(kc p) w -> p kc w", p=128), [], [(CT3, None)])
        act(SCT.t[:], CT3.t[:], AF.Silu, [(CT3, None)], [(SCT, None)])
        WM = [sb("WM%d" % i, [128, KC, 512], BF16) for i in range(2)]
        BMT = sb("BMT", [128, 72])
        for l in cfg.layers:
            dma("sp", BMT.t[:], bmodT[l], [], [(BMT, None)])
            for piece in range(18):
                wmb = WM[piece % 2]
                dma("pool", wmb.t[:], w_mod[l].rearrange("(kc p) n -> p kc n", p=128)[:, :, piece * 512:(piece + 1) * 512],
                    [], [(wmb, None)])
                for oc4 in range(4):
                    oc = piece * 4 + oc4
                    for kc in range(KC):
                        mm(PS[0], PS[0].t[:, oc * 3:oc * 3 + 3], wmb.t[:, kc, oc4 * 128:(oc4 + 1) * 128], SCT.t[:, kc, :],
                           kc == 0, kc == KC - 1, [(wmb, None), (SCT, None)])
            psv = PS[0].t[:, 0:216].rearrange("p (o w) -> p o w", w=3)
            for w in range(3):
                tt("dve", MODT.t[:, l, :, w], psv[:, :, w], BMT.t[:], ALU.add, [(PS[0], None), (BMT, None)], [(MODT, None)])
            for j in range(3):
                for w in range(3):
                    stt(ACOEF.t[:, l, j, w, :], MODT.t[:, l, (3 * j + 1) * 8:(3 * j + 2) * 8, w], 1.0,
                        NWT.t[:, (l * 3 + j) * 8:(l * 3 + j + 1) * 8], ALU.add, ALU.mult,
                        [(MODT, None), (NWT, None)], [(ACOEF, None)])
                    ts("dve", GCOEF.t[:, l, j, w, :], MODT.t[:, l, (3 * j + 2) * 8:(3 * j + 3) * 8, w],
                       (1.0 if j == 1 else 0.5), None, ALU.mult, None, [(MODT, None)], [(GCOEF, None)])

        def shiftc(l, j, w, c):
            return MODT.t[:, l, 3 * j * 8 + c, w:w + 1]

        def rstd_to_psum(src_sq_aps, mean_mat, psa, psb, n, eps, rd):
            k = len(src_sq_aps)
            for i, a in enumerate(src_sq_aps):
                mm(psa, psa.t[:, :n], mean_mat.t[:], a, i == 0, i == k - 1, rd + [(mean_mat, None)])
            act(LNT.t[:, :n], psa.t[:, :n], AF.Ln, [(psa, None)], [(LNT, None)], bias=eps)
            act(psb.t[:, :n], LNT.t[:, :n], AF.Exp, [(LNT, None)], [(psb, None)], scale=-0.5)

        def make_u_tile(l, j, seq, ti, out_fn=None):
            t0_, n_, isctx = cfg.tiles[ti]
            w = 2 if isctx else seq
            for t0 in range(t0_, t0_ + n_, 256):
                n = min(256, t0_ + n_ - t0)
                act(SQ.t[:, :, :n], H.t[:, :, t0:t0 + n], AF.Square, [(H, ti)], [(SQ, None)])
                rstd_to_psum([SQ.t[:, kc, :n] for kc in range(KC)], M1024, PS[6], PS[7], n, RMS_EPS, [(SQ, None)])
                for c in range(KC):
                    tb = TMPU[c % 2]
                    tt("dve", tb.t[:, :n], H.t[:, c, t0:t0 + n], PS[7].t[:, :n], ALU.mult, [(H, ti), (PS[7], None)], [(tb, None)])
                    if out_fn is None:
                        act(U.t[:, c, t0:t0 + n], tb.t[:, :n], AF.Identity, [(tb, None), (ACOEF, None), (MODT, None)], [(U, ti)],
                            bias=shiftc(l, j, w, c), scale=ACOEF.t[:, l, j, w, c:c + 1])
                    else:
                        out_fn(ti, t0, n, isctx, c, tb)

        def make_u(l, j, seq, out_fn=None):
            for ti in range(len(cfg.tiles)):
                make_u_tile(l, j, seq, ti, out_fn)

        groups = [(g * 4, min(4, NFF - g * 4)) for g in range((NFF + 3) // 4)]

        def ffn(l, f, j, seq, do_ctx=True):
            phase()
            WI = [sb("WI%d" % i, [128, KC, 2, 512], BF16) for i in range(2)]
            WO = [sb("WO%d" % i, [128, 4, D], BF16) for i in range(2)]
            SG = [sb("SG%d" % i, [128, 512]) for i in range(2)]
            AT = [sb("AT%d" % i, [128, 4, 512], BF16) for i in range(2)]
            tiles = [t for t in enumerate(cfg.tiles) if do_ctx or not t[1][2]]
            for gidx, (j0, ng) in enumerate(groups):
                wi, wo = WI[gidx % 2], WO[gidx % 2]
                src = w_in[l, f].rearrange("(kc p) (two n) -> p kc two n", p=128, two=2)[:, :, :, j0 * 128:(j0 + ng) * 128]
                for two in range(2):
                    dma("pool", wi.t[:, :, two, :ng * 128], src[:, :, two, :], [], [(wi, None)])
                dma("pool", wo.t[:, :ng, :], w_out[l, f].rearrange("(jj p) d -> p jj d", p=128)[:, j0:j0 + ng, :], [], [(wo, None)])

                def up(ti, t0, n, par):
                    at = AT[par]
                    for q in range(ng):
                        pg, pu = PS[q % 2], PS[2 + q % 2]
                        for kc in range(KC):
                            mm(pg, pg.t[:, :n], wi.t[:, kc, 0, q * 128:(q + 1) * 128], U.t[:, kc, t0:t0 + n], kc == 0, kc == KC - 1,
                               [(wi, None), (U, ti)])
                        for kc in range(KC):
                            mm(pu, pu.t[:, :n], wi.t[:, kc, 1, q * 128:(q + 1) * 128], U.t[:, kc, t0:t0 + n], kc == 0, kc == KC - 1,
                               [(wi, None), (U, ti)])
                        sg = SG[q % 2]
                        act(sg.t[:, :n], pg.t[:, :n], AF.Silu, [(pg, None)], [(sg, None)])
                        tt("dve", at.t[:, q, :n], sg.t[:, :n], pu.t[:, :n], ALU.mult, [(sg, None), (pu, None)], [(at, q)])

                def down(ti, t0, n, isctx, par):
                    at = AT[par]
                    w = 2 if isctx else seq
                    for c in range(KC):
                        py = PS[4 + c % 2]
                        for q in range(ng):
                            mm(py, py.t[:, :n], wo.t[:, q, c * 128:(c + 1) * 128], at.t[:, q, :n], q == 0, q == ng - 1,
                               [(wo, None), (at, q)])
                        stt(H.t[:, c, t0:t0 + n], py.t[:, :n], GCOEF.t[:, l, j, w, c:c + 1], H.t[:, c, t0:t0 + n], ALU.mult, ALU.add,
                            [(py, None), (GCOEF, None), (H, ti)], [(H, ti)])

                if gidx == 0:
                    for k_ in range(min(2, len(tiles))):
                        make_u_tile(l, j, seq, tiles[k_][0])
                for i, (ti, (t0, n, isctx)) in enumerate(tiles):
                    if gidx == 0 and i + 2 < len(tiles):
                        make_u_tile(l, j, seq, tiles[i + 2][0])
                    if i == 0:
                        up(ti, t0, n, 0)
                    if i + 1 < len(tiles):
                        ti2, (t02, n2, _) = tiles[i + 1]
                        up(ti2, t02, n2, (i + 1) % 2)
                    down(ti, t0, n, isctx, i % 2)

        def attention(l, seq, need_ctx):
            phase()
            W5 = sb("W5", [128, KC, 5, 128], BF16)
            WOH = [sb("WOH%d" % i, [128, D], BF16) for i in range(2)]
            QTM = [sb("QT%d" % i, [128, S], BF16) for i in range(2)]
            KT = sb("KT", [128, S], BF16)
            for m_ in range(2):
                memset("pool", QTM[m_], QTM[m_].t[(1 - m_) * 64:(2 - m_) * 64, :], 0.0)
            VT = sb("VT", [128, NCH, 128], BF16)
            COS = sb("COS", [128, NLAT])
            SINS = sb("SINS", [128, NLAT])
            LAMV = sb("LAMV", [128, 256])
            LAMP = sb("LAMP", [128, 128])
            LAMS = sb("LAMS", [128, 8])
            SUBW = sb("SUBW", [128, NA])
            R1 = [sb("R1_%d" % i, [128, 512]) for i in range(2)]
            PT = [sb("PT%d" % i, [128, 512], BF16) for i in range(3)]
            ON = [sb("ON%d" % i, [128, 512]) for i in range(2)]
            OF = sb("OF", [128, 512])
            OSQ = sb("OSQ", [128, 512], BF16)
            OHT = sb("OHT", [128, 512], BF16)
            dma("sp", COS.t[:], ropeC, [], [(COS, None)])
            dma("sp", SINS.t[:], ropeS, [], [(SINS, None)])
            dma("sp", SUBW.t[:], a_subln, [], [(SUBW, None)])
            ai = l // 2
            lam_init = 0.8 - 0.6 * math.exp(-0.3 * l)
            w = seq
            dma("sp", LAMV.t[:], a_lam[ai].partition_broadcast(128), [], [(LAMV, None)])
            tt("dve", LAMP.t[:, 0:64], LAMV.t[:, 0:64], LAMV.t[:, 64:128], ALU.mult, [(LAMV, None)], [(LAMP, None)])
            tt("dve", LAMP.t[:, 64:128], LAMV.t[:, 128:192], LAMV.t[:, 192:256], ALU.mult, [(LAMV, None)], [(LAMP, None)])
            for i in range(2):
                sch.op("dve", lambda e, i=i: e.tensor_reduce(out=LAMS.t[:, i:i + 1], in_=LAMP.t[:, i * 64:(i + 1) * 64],
                                                             axis=mybir.AxisListType.X, op=ALU.add),
                       reads=[(LAMP, None)], writes=[(LAMS, None)])
            act(LAMS.t[:, 4:6], LAMS.t[:, 0:2], AF.Exp, [(LAMS, None)], [(LAMS, None)])
            tt("dve", LAMS.t[:, 6:7], LAMS.t[:, 5:6], LAMS.t[:, 4:5], ALU.subtract, [(LAMS, None)], [(LAMS, None)])
            ts("dve", LAMS.t[:, 2:3], LAMS.t[:, 6:7], -lam_init, None, ALU.add, None, [(LAMS, None)], [(LAMS, None)])
            ts("dve", LAMS.t[:, 3:4], SUBW.t[:, ai:ai + 1], 1.0 - lam_init, None, ALU.mult, None, [(SUBW, None), (LAMS, None)], [(LAMS, None)])
            NEGLAM = LAMS.t[:, 2:3]
            SW = LAMS.t[:, 3:4]
            qtiles = [t for t in enumerate(cfg.tiles) if need_ctx or not t[1][2]]
            for h in range(8):
                w5 = W5
                woh = WOH[h % 2]
                dma("pool", w5.t[:], a_wext[ai].rearrange("(kc p) h f c -> p kc h f c", p=128)[:, :, h], [], [(w5, None)])
                dma("pool", woh.t[:], a_wo[ai, h * 128:(h + 1) * 128, :], [], [(woh, None)])
                for ti, (t0, n, isctx) in enumerate(cfg.tiles):
                    if h == 0:
                        if ti == 0:
                            make_u_tile(l, 1, seq, 0)
                        if ti + 1 < len(cfg.tiles):
                            make_u_tile(l, 1, seq, ti + 1)
                    for (fi, dst) in ((0, None), (2, KT)):
                        pa, pb = PS[0], PS[1]
                        for kc in range(KC):
                            mm(pa, pa.t[:, :n], w5.t[:, kc, fi, :], U.t[:, kc, t0:t0 + n], kc == 0, kc == KC - 1, [(w5, None), (U, ti)])
                        if isctx:
                            if dst is None:
                                for m_ in range(2):
                                    rows = slice(m_ * 64, (m_ + 1) * 64)
                                    cp("act", QTM[m_].t[rows, t0:t0 + n], pa.t[rows, :n], [(pa, None)], [(QTM[m_], ti)])
                            else:
                                cp("act", dst.t[:, t0:t0 + n], pa.t[:, :n], [(pa, None)], [(dst, ti)])
                        else:
                            for kc in range(KC):
                                mm(pb, pb.t[:, :n], w5.t[:, kc, fi + 1, :], U.t[:, kc, t0:t0 + n], kc == 0, kc == KC - 1, [(w5, None), (U, ti)])
                            l0 = t0 - NCTX
                            tt("dve", R1[0].t[:, :n], pa.t[:, :n], COS.t[:, l0:l0 + n], ALU.mult, [(pa, None), (COS, None)], [(R1[0], None)])
                            tt("dve", R1[1].t[:, :n], pb.t[:, :n], SINS.t[:, l0:l0 + n], ALU.mult, [(pb, None), (SINS, None)], [(R1[1], None)])
                            if dst is None:
                                for m_ in range(2):
                                    rows = slice(m_ * 64, (m_ + 1) * 64)
                                    tt("pool", QTM[m_].t[rows, t0:t0 + n], R1[0].t[rows, :n], R1[1].t[rows, :n], ALU.add,
                                       [(R1[0], None), (R1[1], None)], [(QTM[m_], ti)])
                            else:
                                tt("pool", dst.t[:, t0:t0 + n], R1[0].t[:, :n], R1[1].t[:, :n], ALU.add, [(R1[0], None), (R1[1], None)], [(dst, ti)])
                    for s4 in range(0, n, 128):
                        tc = (t0 + s4) // 128
                        pv = PS[2]
                        for kc in range(KC):
                            mm(pv, pv.t[:, :128], U.t[:, kc, t0 + s4:t0 + s4 + 128], w5.t[:, kc, 4, :], kc == 0, kc == KC - 1, [(w5, None), (U, ti)])
                        cp("act", VT.t[:, tc, :], pv.t[:, :128], [(pv, None)], [(VT, tc)])
                pending = []

                def make_epilogue(ti, t0, n, isctx):
                    wsel = 2 if isctx else w

                    def part1():
                        tt("dve", ON[0].t[:, :n], ON[0].t[:, :n], R1[0].t[:, :n], ALU.mult, [(ON[0], None), (R1[0], None)], [(ON[0], None)])
                        stt(ON[1].t[:, :n], ON[1].t[:, :n], NEGLAM, R1[1].t[:, :n], ALU.mult, ALU.mult, [(ON[1], None), (R1[1], None), (LAMS, None)], [(ON[1], None)])
                        tt("dve", OF.t[:, :n], ON[0].t[:, :n], ON[1].t[:, :n], ALU.add, [(ON[0], None), (ON[1], None)], [(OF, None)])
                        act(OSQ.t[:, :n], OF.t[:, :n], AF.Square, [(OF, None)], [(OSQ, None)])
                        rstd_to_psum([OSQ.t[:, :n]], M128, PS[1], R1[0], n, SUBLN_EPS, [(OSQ, None)])
                        stt(OHT.t[:, :n], OF.t[:, :n], SW, R1[0].t[:, :n], ALU.mult, ALU.mult, [(OF, None), (LAMS, None), (R1[0], None)], [(OHT, None)])

                    def part2():
                        for c in range(KC):
                            py = PS[1 + c % 2]
                            mm(py, py.t[:, :n], woh.t[:, c * 128:(c + 1) * 128], OHT.t[:, :n], True, True, [(woh, None), (OHT, None)])
                            stt(H.t[:, c, t0:t0 + n], py.t[:, :n], GCOEF.t[:, l, 1, wsel, c:c + 1], H.t[:, c, t0:t0 + n], ALU.mult, ALU.add,
                                [(py, None), (GCOEF, None), (H, ti)], [(H, ti)])
                    return [part1, part2]

                for (ti, (t0, n, isctx)) in qtiles:
                    kchunks = list(range(cfg.cchunks)) if isctx else list(range(NCH))
                    for m in range(2):
                        po, psm = PS[3], PS[4]
                        nk = len(kchunks)

                        SB_ = [PS[5], PS[6], PS[7], PS[0]]

                        def s_mm(ki):
                            kc_ = kchunks[ki]
                            pS = SB_[ki % 4]
                            mm(pS, pS.t[:, :n], KT.t[:, kc_ * 128:(kc_ + 1) * 128], QTM[m].t[:, t0:t0 + n],
                               True, True, [(KT, None), (QTM[m], None)])

                        for k0 in range(min(3, nk)):
                            s_mm(k0)
                        for ki, kc_ in enumerate(kchunks):
                            pS = SB_[ki % 4]
                            pt = PT[ki % 3]
                            act(pt.t[:, :n], pS.t[:, :n], AF.Exp, [(pS, None)], [(pt, None)], scale=HD ** -0.5)
                            if ki + 3 < nk:
                                s_mm(ki + 3)
                            mm(po, po.t[:, :n], VT.t[:, kc_, :], pt.t[:, :n], ki == 0, ki == nk - 1, [(VT, None), (pt, None)])
                            mm(psm, psm.t[:, :n], ONESB.t[:], pt.t[:, :n], ki == 0, ki == nk - 1, [(ONESB, None), (pt, None)])
                            if m == 0 and pending and (ki == min(3, nk - 1) or ki == min(9, nk - 1)):
                                pending.pop(0)()
                        cp("dve", ON[m].t[:, :n], po.t[:, :n], [(po, None)], [(ON[m], None)])
                        act(LNT.t[:, :n], psm.t[:, :n], AF.Ln, [(psm, None)], [(LNT, None)])
                        act(R1[m].t[:, :n], LNT.t[:, :n], AF.Exp, [(LNT, None)], [(R1[m], None)], scale=-1.0)
                    while pending:
                        pending.pop(0)()
                    pending.extend(make_epilogue(ti, t0, n, isctx))
                while pending:
                    pending.pop(0)()

        def mlstm(l, seq, need_ctx):
            phase()
            mi = l // 2
            w = seq
            dscale = DH_M ** -0.5
            CONV = sb("CONV", [128, NM * 6 * 16])
            SKIPT = sb("SKIPT", [128, NM * 16])
            MNW = sb("MNW", [128, NM * 16])
            BD = sb("BD", [128, 3, 16, 128], BF16)
            WG = sb("WG", [128, 48, 16], BF16)
            WGC = sb("WGC", [128, 2, 16, 16], BF16)
            BGROW = sb("BGROW", [1, 16])
            GT = sb("GT", [128, NCH, 16])
            NL = sb("NL", [128, 2, NCH, 4])
            IG = sb("IG", [128, 2, NCH, 4])
            GB = sb("GB", [128, 2, NCH, 4])
            GIMB = sb("GIMB", [128, 2, NCH, 4])
            GW = sb("GW", [128, 2, NCH, 4])
            GEB = sb("GEB", [128, 2, NCH, 4])
            GDEC = sb("GDEC", [128, 2, NCH, 4])
            common_off = ar["off"]
            BDT = sb("BDT", [128, 3, 16, 128], BF16)
            dma("sp", CONV.t[:], m_convT, [], [(CONV, None)])
            dma("sp", SKIPT.t[:], m_skipT, [], [(SKIPT, None)])
            dma("sp", MNW.t[:], m_nwT, [], [(MNW, None)])
            cv = lambda tap, fc: CONV.t[:, (mi * 6 + tap) * 16 + fc:(mi * 6 + tap) * 16 + fc + 1]
            dma("pool", BD.t[:], m_bd[mi].rearrange("a c k o -> k a c o"), [], [(BD, None)])
            dma("pool", BDT.t[:], m_bdT[mi].rearrange("a c o k -> o a c k"), [], [(BDT, None)])
            dma("pool", WG.t[:], m_wg[mi].rearrange("(c p) g -> p c g", p=128), [], [(WG, None)])
            dma("sp", BGROW.t[:], m_bg[mi:mi + 1, :], [], [(BGROW, None)])
            for fc in range(16):
                pg = PS[0]
                mm(pg, pg.t[:, fc * 32:fc * 32 + 16], BDT.t[:, 0, fc, :], WG.t[:, fc, :], True, False, [(BDT, None), (WG, None)])
                mm(pg, pg.t[:, fc * 32:fc * 32 + 16], BDT.t[:, 1, fc, :], WG.t[:, 16 + fc, :], False, True, [(BDT, None), (WG, None)])
                mm(pg, pg.t[:, fc * 32 + 16:fc * 32 + 32], BDT.t[:, 2, fc, :], WG.t[:, 32 + fc, :], True, True, [(BDT, None), (WG, None)])
            pgv = PS[0].t[:, :].rearrange("p (c two g) -> p two c g", two=2, g=16)
            for two in range(2):
                cp("dve", WGC.t[:, two, :, :], pgv[:, two, :, :], [(PS[0], None)], [(WGC, None)])
            phase()
            ar["off"] = common_off
            WUP = [sb("WUP%d" % i, [128, KC, 128], BF16) for i in range(2)]
            XINB = [sb("XINB%d" % i, [128, S], BF16) for i in range(2)]
            XCB = [sb("XCB%d" % i, [128, S], BF16) for i in range(2)]
            DG = [sb("DG%d" % i, [128, 5, 128], BF16) for i in range(2)]
            VB = sb("VB", [128, NCH, 512], BF16)
            segs = [(0, NCTX), (NCTX, S)]
            for fc in range(16):
                head, fi = fc // 4, fc % 4
                wu, xinb, xcb, dg = WUP[fc % 2], XINB[fc % 2], XCB[fc % 2], DG[fc % 2]
                dma("pool", wu.t[:], m_wup[mi].rearrange("(kc p) n -> p kc n", p=128)[:, :, fc * 128:(fc + 1) * 128], [], [(wu, None)])
                for tap in range(5):
                    ts("pool" if tap % 2 else "dve", dg.t[:, tap, :], IDENT, cv(tap, fc), None, ALU.mult, None, [(CST, None), (CONV, None)], [(dg, None)])
                for ti, (t0, n, isctx) in enumerate(cfg.tiles):
                    if fc == 0:
                        if ti == 0:
                            make_u_tile(l, 1, seq, 0)
                        if ti + 1 < len(cfg.tiles):
                            make_u_tile(l, 1, seq, ti + 1)
                    px = PS[ti % 2]
                    for kc in range(KC):
                        mm(px, px.t[:, :n], wu.t[:, kc, :], U.t[:, kc, t0:t0 + n], kc == 0, kc == KC - 1, [(wu, None), (U, ti)])
                    cp("act", xinb.t[:, t0:t0 + n], px.t[:, :n], [(px, None)], [(xinb, ti)])
                for ti, (t0, n, isctx) in enumerate(cfg.tiles):
                    s0, s1 = segs[0] if isctx else segs[1]
                    pc = PS[2 + ti % 2]
                    taps = [2, 0, 1, 3, 4]
                    for k_, tap in enumerate(taps):
                        d = tap - 2
                        a0, a1 = max(t0, s0 - d), min(t0 + n, s1 - d)
                        mm(pc, pc.t[:, a0 - t0:a1 - t0], dg.t[:, tap, :], xinb.t[:, a0 + d:a1 + d], k_ == 0, k_ == 4, [(dg, None), (xinb, None)])
                    act(xcb.t[:, t0:t0 + n], pc.t[:, :n], AF.Silu, [(pc, None), (CONV, None)], [(xcb, ti)], bias=cv(5, fc))
                dma("sp", xcs[fc], xcb.t[:], [(xcb, None)], [(XCS, fc)])
                for tc in range(NCH):
                    pv = PS[4 + tc % 2]
                    mm(pv, pv.t[:, :128], xinb.t[:, tc * 128:(tc + 1) * 128], BD.t[:, 2, fc, :], True, True, [(xinb, None), (BD, None)])
                    cp("dve", VB.t[:, tc, fi * 128:(fi + 1) * 128], pv.t[:, :128], [(pv, None)], [(VB, None)])
                    pgt = PS[6]
                    first = True
                    if fc == 0:
                        mm(pgt, pgt.t[:, tc * 16:(tc + 1) * 16], ONEROW.t[:], BGROW.t[:], True, False, [(ONEROW, None), (BGROW, None)])
                        first = False
                    mm(pgt, pgt.t[:, tc * 16:(tc + 1) * 16], xcb.t[:, tc * 128:(tc + 1) * 128], WGC.t[:, 0, fc, :], first, False, [(xcb, None), (WGC, None)])
                    mm(pgt, pgt.t[:, tc * 16:(tc + 1) * 16], xinb.t[:, tc * 128:(tc + 1) * 128], WGC.t[:, 1, fc, :], False, True, [(xinb, None), (WGC, None)])
                gtv = GT.t[:].rearrange("p c g -> p (c g)")
                if fc == 0:
                    cp("dve", gtv, PS[6].t[:, :NCH * 16], [(PS[6], None)], [(GT, None)])
                else:
                    tt("dve", gtv, gtv, PS[6].t[:, :NCH * 16], ALU.add, [(PS[6], None), (GT, None)], [(GT, None)])
                if fi == 3:
                    dma("sp", vs[head], VB.t[:], [(VB, None)], [(VS, head)])
            for d_ in range(2):
                cp("dve", IG.t[:, d_, :, :], GT.t[:, :, d_ * 8:d_ * 8 + 4], [(GT, None)], [(IG, None)])
                act(NL.t[:, d_, :, :], GT.t[:, :, d_ * 8 + 4:d_ * 8 + 8], AF.Exp, [(GT, None)], [(NL, None)], scale=-1.0)
            nlall = NL.t[:].rearrange("p a c h -> p (a c h)")
            act(nlall, nlall, AF.Ln, [(NL, None)], [(NL, None)], bias=1.0)
            for d_ in range(2):
                tri = TRIF if d_ == 0 else TRIB
                fl = lambda B_: B_.t[:, d_, :, :].rearrange("p c h -> p (c h)")
                pb_, pt_ = PS[0], PS[1]
                mm(pb_, pb_.t[:, :NCH * 4], tri, fl(NL), True, True, [(CST, None), (NL, None)])
                mm(pt_, pt_.t[:, :NCH * 4], ONES32.t[:], fl(NL), True, True, [(ONES32, None), (NL, None)])
                cp("dve", fl(GB), pb_.t[:, :NCH * 4], [(pb_, None)], [(GB, None)])
                tt("dve", fl(GIMB), fl(IG), fl(GB), ALU.add, [(IG, None), (GB, None)], [(GIMB, None)])
                tt("dve", fl(GW), fl(GIMB), pt_.t[:, :NCH * 4], ALU.subtract, [(GIMB, None), (pt_, None)], [(GW, None)])
                act(fl(GW), fl(GW), AF.Exp, [(GW, None)], [(GW, None)])
                ts("dve", fl(GW), fl(GW), dscale, None, ALU.mult, None, [(GW, None)], [(GW, None)])
                act(fl(GIMB), fl(GIMB), AF.Exp, [(GIMB, None)], [(GIMB, None)])
                act(fl(GEB), fl(GB), AF.Exp, [(GB, None)], [(GEB, None)], scale=-1.0)
                act(fl(GDEC), pt_.t[:, :NCH * 4], AF.Exp, [(pt_, None)], [(GDEC, None)], scale=-1.0)
            for head in range(NH_M):
                phase()
                ar["off"] = common_off
                ST = []
                for d_ in range(2):
                    st = dict(
                        XCC=[sb("XCC%d" % i, [128, 4, 128], BF16) for i in range(2)],
                        VC=[sb("VC%d" % i, [128, 512], BF16) for i in range(2)],
                        HB=[sb("HB%d" % i, [128, 512]) for i in range(2)],
                        CTS=sb("CTS", [128, 4, 512]), CTB=sb("CTB", [128, 4, 512], BF16),
                        NV=sb("NV", [128, 4]), NVB=sb("NVB", [128, 4], BF16),
                        QC=sb("QC", [128, 4, 128], BF16), KCB=sb("KCB", [128, 4, 128], BF16), KW=sb("KW", [128, 512], BF16),
                        SD=sb("SD", [128, 128], BF16), DEN=sb("DEN", [128, 8]),
                        X=[PS[4 * d_], PS[4 * d_ + 1]], Y=PS[4 * d_ + 2], N=PS[4 * d_ + 3])
                    ST.append(st)

                def scan(d_):
                    st = ST[d_]
                    order = list(range(NCH)) if d_ == 0 else (list(range(cfg.cchunks - 1, -1, -1)) + list(range(NCH - 1, cfg.cchunks - 1, -1)))
                    mask = TRIF if d_ == 0 else TRIB
                    CTS, CTB, NV, NVB, QC, KCB, KW, SD, DEN = (st[k] for k in ("CTS", "CTB", "NV", "NVB", "QC", "KCB", "KW", "SD", "DEN"))
                    X0, X1, Y, N_ = st["X"][0], st["X"][1], st["Y"], st["N"]
                    memset("pool", CTS, CTS.t[:], 0.0)
                    memset("pool", CTB, CTB.t[:], 0.0)
                    memset("pool", NV, NV.t[:], 0.0)
                    memset("pool", NVB, NVB.t[:], 0.0)
                    yield
                    for oi, tc in enumerate(order):
                        col = lambda B_: B_.t[:, d_, tc, head:head + 1]
                        xcc, vc, hb = st["XCC"][oi % 2], st["VC"][oi % 2], st["HB"][oi % 2]
                        dma("sp", xcc.t[:], xcs[head * 4:(head + 1) * 4, :, tc * 128:(tc + 1) * 128].rearrange("c p s -> p c s"),
                            [(XCS, None)], [(xcc, None)])
                        dma("sp", vc.t[:], vs[head, :, tc, :], [(VS, head)], [(vc, None)])
                        for i in range(4):
                            mm(X0, X0.t[:, i * 128:(i + 1) * 128], BD.t[:, 0, head * 4 + i, :], xcc.t[:, i, :], True, True, [(BD, None), (xcc, None)])
                        cp("act", QC.t[:].rearrange("p a b -> p (a b)"), X0.t[:, :], [(X0, None)], [(QC, None)])
                        for i in range(4):
                            mm(X1, X1.t[:, i * 128:(i + 1) * 128], BD.t[:, 1, head * 4 + i, :], xcc.t[:, i, :], True, True, [(BD, None), (xcc, None)])
                        ts("dve", KCB.t[:].rearrange("p a b -> p (a b)"), X1.t[:, :], dscale, None, ALU.mult, None, [(X1, None)], [(KCB, None)])
                        yield
                        for i in range(4):
                            mm(X0, X0.t[:, i * 128:(i + 1) * 128], xcc.t[:, i, :], BD.t[:, 1, head * 4 + i, :], True, True, [(BD, None), (xcc, None)])
                        act(KW.t[:], X0.t[:, :], AF.Identity, [(X0, None), (GW, None)], [(KW, None)], scale=col(GW))
                        for i in range(4):
                            mm(Y, Y.t[:, :128], KCB.t[:, i, :], QC.t[:, i, :], i == 0, i == 3, [(KCB, None), (QC, None)])
                        stt(SD.t[:], Y.t[:, :128], col(GIMB), mask, ALU.mult, ALU.mult, [(Y, None), (GIMB, None), (CST, None)], [(SD, None)])
                        yield
                        mm(N_, N_.t[:, :], SD.t[:], vc.t[:], True, False, [(SD, None), (vc, None)])
                        for i in range(4):
                            mm(N_, N_.t[:, :], QC.t[:, i, :], CTB.t[:, i, :], False, i == 3, [(QC, None), (CTB, None)])
                        mm(Y, Y.t[:, 256:257], SD.t[:], ONESB.t[:, 0:1], True, False, [(SD, None), (ONESB, None)])
                        for i in range(4):
                            mm(Y, Y.t[:, 256:257], QC.t[:, i, :], NVB.t[:, i:i + 1], False, i == 3, [(QC, None), (NVB, None)])
                        act(DEN.t[:, 0:1], Y.t[:, 256:257], AF.Abs, [(Y, None), (GEB, None)], [(DEN, None)], scale=col(GEB))
                        ts("dve", DEN.t[:, 1:2], DEN.t[:, 0:1], 1.0, None, ALU.max, None, [(DEN, None)], [(DEN, None)])
                        sch.op("dve", lambda e: e.reciprocal(out=DEN.t[:, 2:3], in_=DEN.t[:, 1:2]), reads=[(DEN, None)], writes=[(DEN, None)])
                        tt("dve", DEN.t[:, 3:4], DEN.t[:, 2:3], col(GEB), ALU.mult, [(DEN, None), (GEB, None)], [(DEN, None)])
                        act(hb.t[:], N_.t[:, :], AF.Identity, [(N_, None), (DEN, None)], [(hb, None)], scale=DEN.t[:, 3:4])
                        dma("sp", hfs[d_, tc], hb.t[:], [(hb, None)], [(HFS, (d_, tc))])
                        yield
                        for i in range(4):
                            pu = st["X"][(i + 1) % 2]
                            mm(pu, pu.t[:, :], KW.t[:, i * 128:(i + 1) * 128], vc.t[:], True, True, [(KW, None), (vc, None)])
                            stt(CTS.t[:, i, :], CTS.t[:, i, :], col(GDEC), pu.t[:, :], ALU.mult, ALU.add, [(CTS, i), (GDEC, None), (pu, None)], [(CTS, i)])
                            cp("act", CTB.t[:, i, :], CTS.t[:, i, :], [(CTS, i)], [(CTB, i)])
                            if i % 2 == 1:
                                yield
                        for i in range(4):
                            mm(Y, Y.t[:, 264 + 2 * i:265 + 2 * i], KW.t[:, i * 128:(i + 1) * 128], ONESB.t[:, 0:1], True, True, [(KW, None), (ONESB, None)])
                        pnv = Y.t[:, 264:272].rearrange("p (a b) -> p a b", b=2)[:, :, 0]
                        stt(NV.t[:], NV.t[:], col(GDEC), pnv, ALU.mult, ALU.add, [(NV, None), (GDEC, None), (Y, None)], [(NV, None)])
                        cp("dve", NVB.t[:], NV.t[:], [(NV, None)], [(NVB, None)])
                        yield

                gens = [scan(0), scan(1)]
                alive = [True, True]
                while any(alive):
                    for gi, g in enumerate(gens):
                        if alive[gi]:
                            try:
                                next(g)
                            except StopIteration:
                                alive[gi] = False
                phase()
                ar["off"] = common_off
                WUZ = sb("WUZ", [128, KC, 512], BF16)
                WD = sb("WD", [128, 4, D], BF16)
                XCO = [sb("XCO%d" % i, [128, 4, 256], BF16) for i in range(2)]
                HF = [sb("HF%d" % i, [128, 2, 512]) for i in range(2)]
                HBW = [sb("HBW%d" % i, [128, 2, 512]) for i in range(2)]
                BNS_ = [sb("BNS%d" % i, [128, 2, 8]) for i in range(2)]
                RS_ = [sb("RS%d" % i, [128, 4]) for i in range(2)]
                TN_ = [sb("TN%d" % i, [128, 4, 256]) for i in range(2)]
                SZ = sb("SZ", [128, 4, 256])
                MT = sb("MT", [128, 4, 256], BF16)
                dma("pool", WUZ.t[:], m_wup[mi].rearrange("(kc p) n -> p kc n", p=128)[:, :, INNER + head * 512:INNER + (head + 1) * 512],
                    [], [(WUZ, None)])
                dma("pool", WD.t[:], m_wdown[mi].rearrange("(c p) d -> p c d", p=128)[:, head * 4:(head + 1) * 4, :], [], [(WD, None)])
                ogroups = []
                for (c0, c1) in ((0, cfg.cchunks), (cfg.cchunks, NCH)):
                    if c0 == 0 and not need_ctx:
                        continue
                    tc = c0
                    while tc < c1:
                        ogroups.append(list(range(tc, min(tc + 2, c1))))
                        tc += 2
                for gi_, grp in enumerate(ogroups):
                    par = gi_ % 2
                    ng = len(grp)
                    T = 128 * ng
                    g0 = grp[0]
                    tok = slice(g0 * 128, g0 * 128 + T)
                    xco, hf, hbw, BNS, RS, TN = XCO[par], HF[par], HBW[par], BNS_[par], RS_[par], TN_[par]
                    isctx = g0 < cfg.cchunks
                    ti = [k for k, (a, n_, _) in enumerate(cfg.tiles) if a <= g0 * 128 < a + n_][0]
                    wsel = 2 if isctx else w
                    dma("sp", xco.t[:, :, :T], xcs[head * 4:(head + 1) * 4, :, g0 * 128:g0 * 128 + T].rearrange("c p s -> p c s"), [(XCS, None)], [(xco, None)])
                    dma("sp", hf.t[:, :ng, :], hfs[0, g0:g0 + ng].rearrange("c p f -> p c f"), [(HFS, None)], [(hf, None)])
                    dma("sp", hbw.t[:, :ng, :], hfs[1, g0:g0 + ng].rearrange("c p f -> p c f"), [(HFS, None)], [(hbw, None)])
                    tt("pool", hf.t[:, :ng, :], hf.t[:, :ng, :], hbw.t[:, :ng, :], ALU.add, [(hf, None), (hbw, None)], [(hf, None)])
                    tt("pool", TN.t[:, :, :T], xco.t[:, :, :T], SKIPT.t[:, mi * 16 + head * 4:mi * 16 + head * 4 + 4].unsqueeze(2).to_broadcast([128, 4, T]), ALU.mult,
                       [(xco, None), (SKIPT, None)], [(TN, None)])
                    pzb = lambda i: PS[i // 2].t[:, (i % 2) * 256:(i % 2) * 256 + T]
                    phb = lambda i: PS[2 + i // 2].t[:, (i % 2) * 256:(i % 2) * 256 + T]
                    for i in range(4):
                        for kc in range(KC):
                            mm(PS[i // 2], pzb(i), WUZ.t[:, kc, i * 128:(i + 1) * 128], U.t[:, kc, tok], kc == 0, kc == KC - 1, [(WUZ, None), (U, ti)])
                    for j in range(ng):
                        sch.op("dve", lambda e, hf=hf, BNS=BNS, j=j: e.bn_stats(out=BNS.t[:, j, 0:6], in_=hf.t[:, j, :]), reads=[(hf, None)], writes=[(BNS, None)])
                        sch.op("dve", lambda e, BNS=BNS, j=j: e.bn_aggr(out=BNS.t[:, j, 6:8], in_=BNS.t[:, j, 0:6]), reads=[(BNS, None)], writes=[(BNS, None)])
                    act(RS.t[:, 0:ng], BNS.t[:, 0:ng, 7], AF.Ln, [(BNS, None)], [(RS, None)], bias=HEAD_LN_EPS)
                    act(RS.t[:, 0:ng], RS.t[:, 0:ng], AF.Exp, [(RS, None)], [(RS, None)], scale=-0.5)
                    for j in range(ng):
                        ts("dve", hf.t[:, j, :], hf.t[:, j, :], BNS.t[:, j, 6:7], RS.t[:, j:j + 1], ALU.subtract, ALU.mult, [(hf, None), (BNS, None), (RS, None)], [(hf, None)])
                    for b in range(2):
                        pzv = PS[b].t[:, 0:512].rearrange("p (a t) -> p a t", a=2)[:, :, :T]
                        szv = SZ.t[:, 2 * b:2 * b + 2, :T]
                        act(szv, pzv, AF.Exp, [(PS[b], None)], [(SZ, b)], scale=-1.0)
                        act(szv, szv, AF.Ln, [(SZ, b)], [(SZ, b)], bias=1.0)
                        act(szv, szv, AF.Exp, [(SZ, b)], [(SZ, b)], scale=-1.0)
                        stt(szv, pzv, 1.0, szv, ALU.mult, ALU.mult, [(PS[b], None), (SZ, b)], [(SZ, b)])
                    for j in range(ng):
                        for i in range(4):
                            tr(PS[2 + i // 2], PS[2 + i // 2].t[:, (i % 2) * 256 + j * 128:(i % 2) * 256 + (j + 1) * 128],
                               hf.t[:, j, i * 128:(i + 1) * 128], IDENT, [(hf, None), (CST, None)])
                    for i in range(4):
                        fcg = mi * 16 + head * 4 + i
                        stt(TN.t[:, i, :T], phb(i), MNW.t[:, fcg:fcg + 1], TN.t[:, i, :T], ALU.mult, ALU.add,
                            [(PS[2 + i // 2], None), (MNW, None), (TN, None)], [(TN, None)])
                    tt("dve", MT.t[:, :, :T], TN.t[:, :, :T], SZ.t[:, :, :T], ALU.mult, [(TN, None), (SZ, None)], [(MT, None)])
                    for c in range(KC):
                        py = PS[4 + c % 4]
                        for i in range(4):
                            mm(py, py.t[:, :T], WD.t[:, i, c * 128:(c + 1) * 128], MT.t[:, i, :T], i == 0, i == 3, [(WD, None), (MT, None)])
                        stt(H.t[:, c, tok], py.t[:, :T], GCOEF.t[:, l, 1, wsel, c:c + 1], H.t[:, c, tok], ALU.mult, ALU.add,
                            [(py, None), (GCOEF, None), (H, ti)], [(H, ti)])

        for seq in range(cfg.nseq):
            phase()
            for ti, (t0, n, isctx) in enumerate(cfg.tiles):
                dma("sp", H.t[:, :, t0:t0 + n], xT[seq].rearrange("(kc p) s -> p kc s", p=128)[:, :, t0:t0 + n], [], [(H, ti)])
            for l in cfg.layers:
                last = (l == cfg.depth - 1)
                ffn(l, 0, 0, seq, True)
                if l % 2 == 0:
                    attention(l, seq, not last)
                else:
                    mlstm(l, seq, not last)
                ffn(l, 1, 2, seq, not last)
            phase()
            OUTB = [sb("OUTB%d" % i, [128, 256]) for i in range(2)]
            cnt = [0]

            def fin(ti, t0, n, isctx, c, tb):
                if cfg.final and isctx:
                    return
                ob = OUTB[cnt[0] % 2]
                cnt[0] += 1
                if cfg.final:
                    ts("dve", ob.t[:, :n], tb.t[:, :n], FNW.t[:, c:c + 1], None, ALU.mult, None, [(tb, None), (FNW, None)], [(ob, None)])
                    dma("sp", outT[seq, c * 128:(c + 1) * 128, t0 - NCTX:t0 - NCTX + n], ob.t[:, :n], [(ob, None)], [])
            if cfg.final:
                make_u(0, 0, seq, out_fn=fin)
            else:
                for ti, (t0, n, isctx) in enumerate(cfg.tiles):
                    dma("sp", outT[seq].rearrange("(kc p) s -> p kc s", p=128)[:, :, t0:t0 + n], H.t[:, :, t0:t0 + n], [(H, ti)], [])
        sch.emit()
    return nc


def _rope_tables(nlat):
    rows = nlat // GRID_W
    row_pos = np.repeat(np.arange(rows, dtype=np.float32), GRID_W)
    col_pos = np.tile(np.arange(GRID_W, dtype=np.float32), rows)
    half = HD // 2
    inv_freq = (1.0 / (ROPE_THETA ** (np.arange(0, half, 2, dtype=np.float32) / half))).astype(np.float32)
    C = np.zeros((128, nlat), np.float32)
    Sg = np.zeros((128, nlat), np.float32)
    for p in range(128):
        d = p % 64
        axis, r, fq = d // 32, (d % 32) // 16, d % 16
        ang = (row_pos if axis == 0 else col_pos) * inv_freq[fq]
        C[p] = np.cos(ang)
        Sg[p] = np.sin(ang) * (-1.0 if r == 0 else 1.0)
    return C, Sg


def _consts():
    s = np.arange(128)[:, None]
    t = np.arange(128)[None, :]
    ident = np.eye(128, dtype=np.float32)
    triF = (s <= t).astype(np.float32)
    triB = (s >= t).astype(np.float32)
    mF = np.where(s <= t, 0.0, -NEG).astype(np.float32)
    mB = np.where(s >= t, 0.0, -NEG).astype(np.float32)
    return np.stack([ident, triF, triB, mF, mB]).astype(np.float32)


def _prep_shared(inp, cfg):
    L = cfg.depth
    f = lambda a: np.ascontiguousarray(np.asarray(a, dtype=np.float32))
    sh = {}
    sh["w_mod"] = f(inp["w_mod"])
    sh["bmodT"] = f(np.asarray(inp["b_mod"]).reshape(L, 72, 128).transpose(0, 2, 1))
    sh["normwT"] = f(np.asarray(inp["norm_w"]).reshape(L, 3, 8, 128).transpose(3, 0, 1, 2).reshape(128, L * 24))
    sh["fnormT"] = f(np.asarray(inp["final_norm_w"]).reshape(8, 128).T)
    sh["ffn_w_in"] = f(inp["ffn_w_in"])
    sh["ffn_w_out"] = f(inp["ffn_w_out"])
    NA, NM = max(cfg.n_attn, 1), max(cfg.n_ml, 1)
    wqkv = np.asarray(inp["attn_w_qkv"], dtype=np.float32)
    na = wqkv.shape[0]
    p = np.arange(128)
    d = p % 64
    partner = np.where((d % 32) < 16, p + 16, p - 16)
    ext = np.zeros((NA, D, 8, 5, 128), np.float32)
    for a in range(min(na, NA)):
        q = wqkv[a][:, 0:D].reshape(D, 8, 128)
        k = wqkv[a][:, D:2 * D].reshape(D, 8, 128)
        v = wqkv[a][:, 2 * D:3 * D].reshape(D, 8, 128)
        ext[a, :, :, 0] = q
        ext[a, :, :, 1] = q[:, :, partner]
        ext[a, :, :, 2] = k
        ext[a, :, :, 3] = k[:, :, partner]
        ext[a, :, :, 4] = v
    sh["a_wext"] = ext
    pad = lambda a, n: f(np.concatenate([np.asarray(a, np.float32)] + [np.asarray(a, np.float32)[:1]] * (n - np.asarray(a).shape[0]), 0)) if np.asarray(a).shape[0] < n else f(np.asarray(a)[:n])
    sh["a_wo"] = pad(inp["attn_w_o"], NA)
    sh["a_lam"] = pad(np.asarray(inp["attn_lambda"]).reshape(-1, 256), NA)
    sh["a_subln"] = f(pad(inp["attn_subln_w"], NA).T)
    C, Sg = _rope_tables(cfg.nlat)
    sh["ropeC"], sh["ropeS"] = C, Sg
    sh["m_wup"] = pad(inp["mlstm_w_up"], NM)
    cw = pad(inp["mlstm_conv_w"], NM)
    cb = pad(inp["mlstm_conv_b"], NM)
    cc = np.concatenate([cw, cb[:, None, :]], 1)
    sh["m_convT"] = f(cc.reshape(NM, 6, 16, 128).transpose(3, 0, 1, 2).reshape(128, NM * 96))
    wq = pad(inp["mlstm_w_qkv"], NM)
    bd = np.zeros((NM, 3, 16, 128, 128), np.float32)
    for c in range(16):
        for g in range(32):
            bd[:, :, c, g * 4:(g + 1) * 4, g * 4:(g + 1) * 4] = wq[:, :, c * 32 + g]
    sh["m_bd"] = bd
    sh["m_bdT"] = f(bd.transpose(0, 1, 2, 4, 3))
    sh["m_wg"] = pad(inp["mlstm_w_gates"], NM)
    sh["m_bg"] = pad(inp["mlstm_b_gates"], NM)
    sh["m_skipT"] = f(pad(inp["mlstm_skip"], NM).reshape(NM, 16, 128).transpose(2, 0, 1).reshape(128, NM * 16))
    sh["m_nwT"] = f(pad(inp["mlstm_norm_w"], NM).reshape(NM, 16, 128).transpose(2, 0, 1).reshape(128, NM * 16))
    sh["m_wdown"] = pad(inp["mlstm_w_down"], NM)
    sh["cst"] = _consts()
    return sh


def run_cfg(inp, cfg, n_cores=8):
    x = np.asarray(inp["x"], np.float32)
    ctx = np.asarray(inp["ctx"], np.float32)
    c = np.asarray(inp["c"], np.float32)
    c_ctx = np.asarray(inp["c_ctx"], np.float32)
    B = x.shape[0]
    assert B == n_cores * cfg.nseq
    sh = _prep_shared(inp, cfg)
    nc = build_program(cfg)
    in_maps = []
    for core in range(n_cores):
        bs = [core * cfg.nseq + i for i in range(cfg.nseq)]
        xt = np.stack([np.concatenate([ctx[b], x[b]], 0).T for b in bs]).astype(np.float32)
        cols = [c[bs[i]] if i < cfg.nseq else c[bs[0]] for i in range(2)] + [c_ctx]
        m = dict(sh)
        m["xT"] = np.ascontiguousarray(xt)
        m["cT"] = np.ascontiguousarray(np.stack(cols, 1).astype(np.float32))
        in_maps.append(m)
    res = run_bass_kernel_spmd(nc, in_maps, core_ids=list(range(n_cores)))
    outs = []
    for core in range(n_cores):
        o = np.asarray(res.results[core]["outT"])
        for i in range(cfg.nseq):
            outs.append(o[i].T)
    return np.ascontiguousarray(np.stack(outs).astype(np.float32))


def kernel(**inputs):
    cfg = Cfg()
    return run_cfg(inputs, cfg)
```
